# Optimizing a Trainium2 kernel written in Bass

```python
import math
import jax, jax.numpy as jnp
from jax import lax
import numpy as np

D_MODEL = 1024
BATCH = 16
SEQ = 2048
DEPTH = 1

CTX_LEN = 256
GRID_W = 64

N_HEADS = 8
QK_NOPE_DIM = 128
QK_ROPE_DIM = 64
V_HEAD_DIM = 128
Q_RANK = 384
KV_RANK = 256
ROPE_AXIS_DIM = QK_ROPE_DIM // 2
ROPE_BASE = 10000.0
Q_BLOCK = 128

S5_WIDTH = D_MODEL // 2
S5_GROUP = 16
S5_GROUPS = S5_WIDTH // S5_GROUP
S5_STATE = 64
S5_DT_MIN = 1e-3
S5_DT_MAX = 1e-1

PEER_HEADS = 8
PEER_N_KEYS = 128
PEER_EXPERTS = PEER_N_KEYS * PEER_N_KEYS
PEER_TOPK = 16
PEER_QUERY_DIM = 256
PEER_HALF = PEER_QUERY_DIM // 2
TOKEN_BLOCK = 128

IN_SPLIT_POINTS = (Q_RANK, Q_RANK + KV_RANK, Q_RANK + KV_RANK + QK_ROPE_DIM, Q_RANK + KV_RANK + QK_ROPE_DIM + S5_WIDTH, Q_RANK + KV_RANK + QK_ROPE_DIM + S5_WIDTH + D_MODEL)
IN_WIDTH = Q_RANK + KV_RANK + QK_ROPE_DIM + S5_WIDTH + 2 * D_MODEL

DEEPNORM_ALPHA = (2.0 * DEPTH) ** 0.25
DEEPNORM_BETA = (8.0 * DEPTH) ** -0.25
LN_EPS = 1e-6
N_MOD = 6

kernel_name = 'hybrid_mla_s5_peer_dit_block'


def layer_norm_plain(x):
    xf = x.astype(jnp.float32)
    xc = xf - jnp.mean(xf, axis=-1, keepdims=True)
    var = jnp.mean(xc * xc, axis=-1, keepdims=True)
    return (xc * lax.rsqrt(var + LN_EPS)).astype(x.dtype)


def layer_norm(x, g, b):
    return layer_norm_plain(x) * g + b


def rms_norm(x, g):
    xf = x.astype(jnp.float32)
    y = xf * lax.rsqrt(jnp.mean(xf * xf, axis=-1, keepdims=True) + LN_EPS)
    return y.astype(x.dtype) * g


def modulation(cond, w_mod, b_mod):
    m = jax.nn.silu(cond) @ w_mod + b_mod
    return jnp.split(m[..., None, :], N_MOD, axis=-1)


def modulate(h, shift, scale):
    return layer_norm_plain(h) * (1.0 + scale) + shift


def axial_rope_tables(rows, dtype):
    row = jnp.broadcast_to(jnp.arange(rows, dtype=jnp.float32)[:, None], (rows, GRID_W)).reshape(-1)
    col = jnp.broadcast_to(jnp.arange(GRID_W, dtype=jnp.float32)[None, :], (rows, GRID_W)).reshape(-1)
    inv_freq = jnp.power(ROPE_BASE, -jnp.arange(0, ROPE_AXIS_DIM, 2, dtype=jnp.float32) / ROPE_AXIS_DIM)
    ang_r = row[:, None, None] * inv_freq
    ang_c = col[:, None, None] * inv_freq
    return (jnp.cos(ang_r).astype(dtype), jnp.sin(ang_r).astype(dtype),
            jnp.cos(ang_c).astype(dtype), jnp.sin(ang_c).astype(dtype))


def rotate_half_pairs(x, cos, sin):
    x1, x2 = jnp.split(x, 2, axis=-1)
    return jnp.concatenate([x1 * cos - x2 * sin, x2 * cos + x1 * sin], axis=-1)


def axial_rope(x, rope):
    cos_r, sin_r, cos_c, sin_c = rope
    x_row, x_col = jnp.split(x, 2, axis=-1)
    return jnp.concatenate([rotate_half_pairs(x_row, cos_r, sin_r), rotate_half_pairs(x_col, cos_c, sin_c)], axis=-1)


def mla_project(cq, ckv, kpe, q_norm_g, kv_norm_g, w_uq, w_ukv, rope):
    b, l, _ = cq.shape
    q = (rms_norm(cq, q_norm_g) @ w_uq).reshape(b, l, N_HEADS, QK_NOPE_DIM + QK_ROPE_DIM)
    kv = (rms_norm(ckv, kv_norm_g) @ w_ukv).reshape(b, l, N_HEADS, QK_NOPE_DIM + V_HEAD_DIM)
    q_nope, q_pe = jnp.split(q, [QK_NOPE_DIM], axis=-1)
    k_nope, v = jnp.split(kv, [QK_NOPE_DIM], axis=-1)
    k_pe = kpe[:, :, None, :]
    if rope is not None:
        q_pe = axial_rope(q_pe, rope)
        k_pe = axial_rope(k_pe, rope)
    k_pe = jnp.broadcast_to(k_pe, (b, l, N_HEADS, QK_ROPE_DIM))
    q = jnp.concatenate([q_nope, q_pe], axis=-1)
    k = jnp.concatenate([k_nope, k_pe], axis=-1)
    return q.transpose(0, 2, 1, 3), k.transpose(0, 2, 1, 3), v.transpose(0, 2, 1, 3)


def block_attention(q, k, v):
    b, h, lq, dk = q.shape
    nblk = lq // Q_BLOCK
    scale = dk ** -0.5
    qb = jnp.moveaxis(q.reshape(b, h, nblk, Q_BLOCK, dk), 2, 0)

    def attend(qi):
        s = jnp.einsum('bhqd,bhkd->bhqk', qi, k).astype(jnp.float32) * scale
        p = jax.nn.softmax(s, axis=-1).astype(v.dtype)
        return jnp.einsum('bhqk,bhkd->bhqd', p, v)

    o = lax.map(attend, qb)
    return jnp.moveaxis(o, 0, 2).reshape(b, h, lq, v.shape[-1])


def s5_discretize(a_re, a_im, log_dt, b_re, b_im):
    a_re = a_re.astype(jnp.float32)
    a_im = a_im.astype(jnp.float32)
    b_re = b_re.astype(jnp.float32)
    b_im = b_im.astype(jnp.float32)
    dt = jnp.exp(log_dt.astype(jnp.float32))[:, None]
    mag = jnp.exp(a_re * dt)
    ab_re = mag * jnp.cos(a_im * dt)
    ab_im = mag * jnp.sin(a_im * dt)
    den = a_re * a_re + a_im * a_im
    f_re = ((ab_re - 1.0) * a_re + ab_im * a_im) / den
    f_im = (ab_im * a_re - (ab_re - 1.0) * a_im) / den
    bb_re = f_re[..., None] * b_re - f_im[..., None] * b_im
    bb_im = f_re[..., None] * b_im + f_im[..., None] * b_re
    return ab_re, ab_im, bb_re, bb_im


def s5_scan(u, ab_re, ab_im, bb_re, bb_im, h0_re, h0_im):
    bu_re = jnp.einsum('blgc,gpc->blgp', u, bb_re)
    bu_im = jnp.einsum('blgc,gpc->blgp', u, bb_im)
    bu_re = bu_re.at[:, 0].add(ab_re * h0_re - ab_im * h0_im)
    bu_im = bu_im.at[:, 0].add(ab_re * h0_im + ab_im * h0_re)
    shape = (1, u.shape[1]) + ab_re.shape
    a_re = jnp.broadcast_to(ab_re, shape)
    a_im = jnp.broadcast_to(ab_im, shape)

    def combine(e1, e2):
        a1r, a1i, b1r, b1i = e1
        a2r, a2i, b2r, b2i = e2
        return (a2r * a1r - a2i * a1i, a2r * a1i + a2i * a1r,
                a2r * b1r - a2i * b1i + b2r, a2r * b1i + a2i * b1r + b2i)

    _, _, h_re, h_im = lax.associative_scan(combine, (a_re, a_im, bu_re, bu_im), axis=1)
    return h_re, h_im


def s5_readout(h_re, h_im, c_re, c_im):
    return jnp.einsum('blgp,gcp->blgc', h_re, c_re) - jnp.einsum('blgp,gcp->blgc', h_im, c_im)


def s5_mixer(u_lat, u_ctx, a_re, a_im, log_dt, b_re, b_im, c_re, c_im, d_skip, need_ctx):
    bsz, l_lat, _ = u_lat.shape
    l_ctx = u_ctx.shape[1]
    ul = u_lat.astype(jnp.float32).reshape(bsz, l_lat, S5_GROUPS, S5_GROUP)
    uc = u_ctx.astype(jnp.float32).reshape(bsz, l_ctx, S5_GROUPS, S5_GROUP)
    dk = d_skip.astype(jnp.float32).reshape(S5_GROUPS, S5_GROUP)
    zeros = jnp.zeros((bsz, S5_GROUPS, S5_STATE), jnp.float32)
    y_lat = dk * ul
    y_ctx = dk * uc if need_ctx else None
    for direction in range(2):
        ab_re, ab_im, bb_re, bb_im = s5_discretize(a_re[direction], a_im[direction], log_dt[direction],
                                                   b_re[direction], b_im[direction])
        cr = c_re[direction].astype(jnp.float32)
        ci = c_im[direction].astype(jnp.float32)
        flip = direction == 1
        uc_d = jnp.flip(uc, axis=1) if flip else uc
        ul_d = jnp.flip(ul, axis=1) if flip else ul
        hc_re, hc_im = s5_scan(uc_d, ab_re, ab_im, bb_re, bb_im, zeros, zeros)
        hl_re, hl_im = s5_scan(ul_d, ab_re, ab_im, bb_re, bb_im, hc_re[:, -1], hc_im[:, -1])
        yl = s5_readout(hl_re, hl_im, cr, ci)
        y_lat = y_lat + (jnp.flip(yl, axis=1) if flip else yl)
        if need_ctx:
            yc = s5_readout(hc_re, hc_im, cr, ci)
            y_ctx = y_ctx + (jnp.flip(yc, axis=1) if flip else yc)
    y_lat = y_lat.reshape(bsz, l_lat, S5_WIDTH).astype(u_lat.dtype)
    if need_ctx:
        y_ctx = y_ctx.reshape(bsz, l_ctx, S5_WIDTH).astype(u_ctx.dtype)
    return y_lat, y_ctx


def branch_merge(att, s5_y, gate_mla, gate_s5, w_glu, w_out):
    b, h, l, dv = att.shape
    att = att.transpose(0, 2, 1, 3).reshape(b, l, h * dv)
    glu_a, glu_b = jnp.split(jax.nn.gelu(s5_y) @ w_glu, 2, axis=-1)
    s5_out = glu_a * jax.nn.sigmoid(glu_b)
    merged = jax.nn.sigmoid(gate_mla) * att + jax.nn.sigmoid(gate_s5) * s5_out
    return merged @ w_out


def peer(h, wq, keys, u_tab, v_tab):
    b, l, d = h.shape
    blocks = h.reshape(-1, TOKEN_BLOCK, d)

    def retrieve(xb):
        q = (xb @ wq).reshape(TOKEN_BLOCK, PEER_HEADS, 2, PEER_HALF)
        s = jnp.einsum('thzd,hznd->thzn', q, keys).astype(jnp.float32)
        sv, si = lax.top_k(s, PEER_TOPK)
        cand_s = (sv[:, :, 0, :, None] + sv[:, :, 1, None, :]).reshape(TOKEN_BLOCK, PEER_HEADS, PEER_TOPK * PEER_TOPK)
        cand_e = (si[:, :, 0, :, None] * PEER_N_KEYS + si[:, :, 1, None, :]).reshape(TOKEN_BLOCK, PEER_HEADS, PEER_TOPK * PEER_TOPK)
        top_s, top_pos = lax.top_k(cand_s, PEER_TOPK)
        experts = jnp.take_along_axis(cand_e, top_pos, axis=-1)
        g = jax.nn.softmax(top_s, axis=-1).astype(xb.dtype)
        act = jax.nn.gelu(jnp.einsum('thkd,td->thk', u_tab[experts], xb))
        return jnp.einsum('thk,thkd->td', g * act, v_tab[experts])

    return lax.map(retrieve, blocks).reshape(b, l, d)


def setup_inputs(seed: int = 0) -> dict:
    key = jax.random.key(seed)
    ks = jax.random.split(key, 29)
    f32 = jnp.float32

    def nrm(i, shape, scale):
        return jax.random.normal(ks[i], shape, f32) * scale

    G, P, C = S5_GROUPS, S5_STATE, S5_GROUP
    n_idx = jnp.arange(P, dtype=f32)
    return {
        'x': nrm(0, (BATCH, SEQ, D_MODEL), 1.0),
        'c': nrm(1, (BATCH, D_MODEL), 1.0),
        'ctx': nrm(2, (BATCH, CTX_LEN, D_MODEL), 1.0),
        'c_ctx': nrm(3, (D_MODEL,), 1.0),
        'w_mod': nrm(4, (DEPTH, D_MODEL, N_MOD * D_MODEL), 0.5 * D_MODEL ** -0.5),
        'b_mod': nrm(5, (DEPTH, N_MOD * D_MODEL), 0.01),
        'w_in': nrm(6, (DEPTH, D_MODEL, IN_WIDTH), D_MODEL ** -0.5),
        'q_norm_g': 1.0 + nrm(7, (DEPTH, Q_RANK), 0.02),
        'kv_norm_g': 1.0 + nrm(8, (DEPTH, KV_RANK), 0.02),
        'w_uq': nrm(9, (DEPTH, Q_RANK, N_HEADS * (QK_NOPE_DIM + QK_ROPE_DIM)), Q_RANK ** -0.5),
        'w_ukv': nrm(10, (DEPTH, KV_RANK, N_HEADS * (QK_NOPE_DIM + V_HEAD_DIM)), KV_RANK ** -0.5),
        's5_a_re': -0.5 + nrm(11, (DEPTH, 2, G, P), 0.01),
        's5_a_im': math.pi * n_idx + nrm(12, (DEPTH, 2, G, P), 0.01),
        's5_log_dt': jax.random.uniform(ks[13], (DEPTH, 2, G), f32, math.log(S5_DT_MIN), math.log(S5_DT_MAX)),
        's5_b_re': nrm(14, (DEPTH, 2, G, P, C), (2.0 * C) ** -0.5),
        's5_b_im': nrm(15, (DEPTH, 2, G, P, C), (2.0 * C) ** -0.5),
        's5_c_re': nrm(16, (DEPTH, 2, G, C, P), 2.0 ** -0.5),
        's5_c_im': nrm(17, (DEPTH, 2, G, C, P), 2.0 ** -0.5),
        's5_d': nrm(18, (DEPTH, S5_WIDTH), 1.0),
        'w_glu': nrm(19, (DEPTH, S5_WIDTH, 2 * D_MODEL), S5_WIDTH ** -0.5),
        'w_out': nrm(20, (DEPTH, D_MODEL, D_MODEL), DEEPNORM_BETA * D_MODEL ** -0.5),
        'ln1_g': 1.0 + nrm(21, (DEPTH, D_MODEL), 0.02),
        'ln1_b': nrm(22, (DEPTH, D_MODEL), 0.02),
        'peer_wq': nrm(23, (DEPTH, D_MODEL, PEER_HEADS * PEER_QUERY_DIM), D_MODEL ** -0.5),
        'peer_keys': nrm(24, (DEPTH, PEER_HEADS, 2, PEER_N_KEYS, PEER_HALF), PEER_HALF ** -0.5),
        'peer_u': nrm(25, (DEPTH, PEER_EXPERTS, D_MODEL), D_MODEL ** -0.5),
        'peer_v': nrm(26, (DEPTH, PEER_EXPERTS, D_MODEL), DEEPNORM_BETA),
        'ln2_g': 1.0 + nrm(27, (DEPTH, D_MODEL), 0.02),
        'ln2_b': nrm(28, (DEPTH, D_MODEL), 0.02),
    }


def reference(x, c, ctx, c_ctx, w_mod, b_mod, w_in, q_norm_g, kv_norm_g, w_uq, w_ukv,
              s5_a_re, s5_a_im, s5_log_dt, s5_b_re, s5_b_im, s5_c_re, s5_c_im, s5_d, w_glu,
              w_out, ln1_g, ln1_b, peer_wq, peer_keys, peer_u, peer_v, ln2_g, ln2_b):
    rows = x.shape[1] // GRID_W
    rope = axial_rope_tables(rows, x.dtype)
    h_lat, h_ctx = x, ctx
    for layer in range(DEPTH):
        need_ctx = layer < DEPTH - 1
        sh_a, sc_a, g_a, sh_f, sc_f, g_f = modulation(c, w_mod[layer], b_mod[layer])
        csh_a, csc_a, cg_a, csh_f, csc_f, cg_f = modulation(c_ctx, w_mod[layer], b_mod[layer])

        z_lat = modulate(h_lat, sh_a, sc_a) @ w_in[layer]
        z_ctx = modulate(h_ctx, csh_a, csc_a) @ w_in[layer]
        cq_l, ckv_l, kpe_l, su_l, gm_l, gs_l = jnp.split(z_lat, IN_SPLIT_POINTS, axis=-1)
        cq_c, ckv_c, kpe_c, su_c, gm_c, gs_c = jnp.split(z_ctx, IN_SPLIT_POINTS, axis=-1)

        q_l, k_l, v_l = mla_project(cq_l, ckv_l, kpe_l, q_norm_g[layer], kv_norm_g[layer], w_uq[layer], w_ukv[layer], rope)
        q_c, k_c, v_c = mla_project(cq_c, ckv_c, kpe_c, q_norm_g[layer], kv_norm_g[layer], w_uq[layer], w_ukv[layer], None)
        att_lat = block_attention(q_l, jnp.concatenate([k_c, k_l], axis=2), jnp.concatenate([v_c, v_l], axis=2))

        s5_lat, s5_ctx = s5_mixer(su_l, su_c, s5_a_re[layer], s5_a_im[layer], s5_log_dt[layer],
                                  s5_b_re[layer], s5_b_im[layer], s5_c_re[layer], s5_c_im[layer],
                                  s5_d[layer], need_ctx)

        out_lat = branch_merge(att_lat, s5_lat, gm_l, gs_l, w_glu[layer], w_out[layer])
        h_lat = layer_norm(DEEPNORM_ALPHA * h_lat + g_a * out_lat, ln1_g[layer], ln1_b[layer])

        if need_ctx:
            att_ctx = block_attention(q_c, k_c, v_c)
            out_ctx = branch_merge(att_ctx, s5_ctx, gm_c, gs_c, w_glu[layer], w_out[layer])
            h_ctx = layer_norm(DEEPNORM_ALPHA * h_ctx + cg_a * out_ctx, ln1_g[layer], ln1_b[layer])
            f_ctx = peer(modulate(h_ctx, csh_f, csc_f), peer_wq[layer], peer_keys[layer], peer_u[layer], peer_v[layer])
            h_ctx = layer_norm(DEEPNORM_ALPHA * h_ctx + cg_f * f_ctx, ln2_g[layer], ln2_b[layer])

        f_lat = peer(modulate(h_lat, sh_f, sc_f), peer_wq[layer], peer_keys[layer], peer_u[layer], peer_v[layer])
        h_lat = layer_norm(DEEPNORM_ALPHA * h_lat + g_f * f_lat, ln2_g[layer], ln2_b[layer])
    return h_lat
```

```python
import math
from contextlib import ExitStack
import numpy as np
import concourse.bass as bass
import concourse.mybir as mybir
from concourse.bass_utils import run_bass_kernel_spmd

F32 = mybir.dt.float32
I32 = mybir.dt.int32
U32 = mybir.dt.uint32
ALU = mybir.AluOpType
AF = mybir.ActivationFunctionType
AX = mybir.AxisListType

COMPUTE = ("vector", "scalar", "gpsimd", "tensor")
ALLENG = ("sync", "scalar", "gpsimd", "vector", "tensor")
EPOCH = 12000
NDSEM = 24

NB = 2
L = 2048
LC = 256
LT = L + LC
D = 1024
T = 128
NCH = LT // T
EPS = 1e-6
ALPHA = (2.0 * 1) ** 0.25
TWO_PI = 6.283185


class Prog:
    def __init__(self, nc, stack):
        self.nc = nc
        self.stack = stack
        self.ops = {e: [] for e in ALLENG}
        self.csem = {}
        self.ccount = {}
        self.nsem = 0
        for e in COMPUTE:
            self._new_csem(e)
        self.dpool = {}
        for pn, cnt in (("hw", 16), ("sw", NDSEM)):
            self.dpool[pn] = {"sems": [stack.enter_context(nc.semaphore(f"dq{pn}{i}")) for i in range(cnt)], "val": [0] * cnt, "next": 0}
        self.known = {e: {} for e in ALLENG}
        self.last_w = {}
        self.readers = {}
        self.ninst = 0
        self.allsems = {}

    def _new_csem(self, e):
        self.nsem += 1
        self.csem[e] = self.stack.enter_context(self.nc.semaphore(f"c_{e}_{self.nsem}"))
        self.ccount[e] = 0

    def _emit(self, eng, fn, reads, writes, dma):
        deps = {}

        def add(tok, kind):
            if tok is None:
                return
            sem, val, peng, pdma = tok
            if (not pdma) and peng == eng and not dma:
                if eng == "tensor":
                    return
            k = id(sem)
            if k not in deps or deps[k][1] < val:
                deps[k] = (sem, val)

        for r in reads:
            add(self.last_w.get(r), "raw")
        for w in writes:
            add(self.last_w.get(w), "waw")
            for tok in self.readers.get(w, {}).values():
                add(tok, "war")
        if dma:
            pool = self.dpool["sw" if eng == "gpsimd" else "hw"]
            i = pool["next"]
            pool["next"] = (i + 1) % len(pool["sems"])
            if pool["val"][i] > 0:
                k = id(pool["sems"][i])
                if k not in deps or deps[k][1] < pool["val"][i]:
                    deps[k] = (pool["sems"][i], pool["val"][i])
            pool["val"][i] += 16
            tok = (pool["sems"][i], pool["val"][i], eng, True)
            inc = 16
        else:
            if self.ccount[eng] >= EPOCH:
                self._new_csem(eng)
            self.ccount[eng] += 1
            tok = (self.csem[eng], self.ccount[eng], eng, False)
            inc = 1
        self.allsems[id(tok[0])] = (tok[0], tok[1])
        waits = []
        kn = self.known[eng]
        for k, (sem, val) in deps.items():
            if kn.get(k, 0) >= val:
                continue
            kn[k] = val
            waits.append((sem, val))
        tsem = tok[0]

        def run(eh, waits=waits, fn=fn, tsem=tsem, inc=inc):
            for sem, val in waits:
                eh.wait_ge(sem, val)
            fn(eh).then_inc(tsem, inc)

        self.ops[eng].append(run)
        self.ninst += 1
        for w in writes:
            self.last_w[w] = tok
            self.readers[w] = {}
        for r in reads:
            self.readers.setdefault(r, {})[id(tok[0])] = tok
        return tok

    def op(self, eng, fn, reads=(), writes=()):
        return self._emit(eng, fn, list(reads), list(writes), False)

    def dma(self, q, out, in_, reads=(), writes=(), **kw):
        return self._emit(q, lambda e: e.dma_start(out=out, in_=in_, **kw), list(reads), list(writes), True)

    def dma_fn(self, q, fn, reads=(), writes=()):
        return self._emit(q, fn, list(reads), list(writes), True)

    def barrier(self):
        snap = dict(self.allsems)
        for eng in ALLENG:
            waits = []
            kn = self.known[eng]
            for k, (sem, val) in snap.items():
                if kn.get(k, 0) < val:
                    kn[k] = val
                    waits.append((sem, val))

            def run(eh, waits=waits):
                for sem, val in waits:
                    eh.wait_ge(sem, val)

            self.ops[eng].append(run)

    def flush(self):
        nc = self.nc
        ops = self.ops
        with nc.Block() as block:
            @block.sync
            def _(e):
                for f in ops["sync"]:
                    f(e)

            @block.scalar
            def _(e):
                for f in ops["scalar"]:
                    f(e)

            @block.gpsimd
            def _(e):
                for f in ops["gpsimd"]:
                    f(e)

            @block.vector
            def _(e):
                for f in ops["vector"]:
                    f(e)

            @block.tensor
            def _(e):
                for f in ops["tensor"]:
                    f(e)
        self.ops = {e: [] for e in ALLENG}


def host_consts():
    f = np.float32
    c = {}
    c["ident"] = np.eye(128, dtype=f)
    rows = L // 64
    row = np.broadcast_to(np.arange(rows, dtype=f)[:, None], (rows, 64)).reshape(-1)
    col = np.broadcast_to(np.arange(64, dtype=f)[None, :], (rows, 64)).reshape(-1)
    inv_freq = np.power(f(10000.0), (-np.arange(0, 32, 2, dtype=f) / f(32))).astype(f)
    ang_r = (row[:, None] * inv_freq[None, :]).astype(f)
    ang_c = (col[:, None] * inv_freq[None, :]).astype(f)
    cr, sr, cc_, sc_ = np.cos(ang_r).astype(f), np.sin(ang_r).astype(f), np.cos(ang_c).astype(f), np.sin(ang_c).astype(f)
    c["ropecos"] = np.ascontiguousarray(np.concatenate([cr.T, cr.T, cc_.T, cc_.T], 0))
    c["ropesin"] = np.ascontiguousarray(np.concatenate([sr.T, sr.T, sc_.T, sc_.T], 0))
    R = np.zeros((64, 64), f)
    I16 = np.eye(16, dtype=f)
    R[0:16, 16:32] = -I16
    R[16:32, 0:16] = I16
    R[32:48, 48:64] = -I16
    R[48:64, 32:48] = I16
    c["ropeRT"] = np.ascontiguousarray(R.T)
    c["ones"] = np.ones((128, 128), f)
    c["iota_s"] = np.broadcast_to(np.arange(T, dtype=f)[None, :], (128, T)).copy()
    c["iota_c"] = np.broadcast_to(np.arange(NCH, dtype=f)[None, :], (128, NCH)).copy()
    m = np.ones((128, LT), f)
    m[:, ::T] = 0.0
    c["cmask"] = m
    p = np.arange(128)
    c["pmask"] = np.stack([((p // 16) % 2 == 0), ((p // 16) % 2 == 1), (p // 64 == 0), (p // 64 == 1)], 1).astype(f)
    c["iota16"] = np.broadcast_to(np.arange(16, dtype=f)[None, None, :], (128, 128, 16)).reshape(128, 2048).copy()
    return c


def layout_inputs(inp, core):
    f = np.float32
    b0 = core * NB
    m = {}
    m["x2"] = np.ascontiguousarray(inp["x"][b0:b0 + NB])
    m["ctx2"] = np.ascontiguousarray(inp["ctx"][b0:b0 + NB])
    cc = np.concatenate([inp["c"][b0:b0 + NB], inp["c_ctx"][None, :]], 0)
    m["ccT"] = np.ascontiguousarray(cc.reshape(3, 8, 128).transpose(2, 1, 0))
    m["w_mod"] = inp["w_mod"][0]
    m["b_mod"] = inp["b_mod"]
    m["w_in"] = inp["w_in"][0]
    m["qg"] = np.ascontiguousarray(inp["q_norm_g"][0].reshape(3, 128).T)
    m["kvg"] = np.ascontiguousarray(inp["kv_norm_g"][0].reshape(2, 128).T)
    m["w_uq"] = inp["w_uq"][0]
    m["w_ukv"] = inp["w_ukv"][0]

    def L1(a):
        return np.ascontiguousarray(a.reshape(2, 16, 2, 64).transpose(2, 3, 0, 1).reshape(128, 2, 16))

    def L2(a):
        t = a.reshape(2, 4, 8, 64).transpose(2, 0, 1, 3)
        return np.ascontiguousarray(np.broadcast_to(t[:, None], (8, 16, 2, 4, 64)).reshape(128, 2, 4, 64))

    are, aim, ldt = inp["s5_a_re"][0], inp["s5_a_im"][0], inp["s5_log_dt"][0]
    ldtf = np.broadcast_to(ldt[:, :, None], (2, 32, 64))
    m["s_are1"], m["s_aim1"], m["s_ldt1"] = L1(are), L1(aim), L1(ldtf)
    m["s_are2"], m["s_aim2"], m["s_ldt2"] = L2(are), L2(aim), L2(ldtf)

    def LB(b):
        return np.ascontiguousarray(b.reshape(2, 4, 8, 64, 16).transpose(2, 4, 0, 1, 3).reshape(128, 2, 4, 64))

    m["s_bre2"], m["s_bim2"] = LB(inp["s5_b_re"][0]), LB(inp["s5_b_im"][0])

    def LC_(c_):
        return np.ascontiguousarray(c_.reshape(2, 16, 2, 16, 64).transpose(2, 4, 0, 1, 3).reshape(128, 2, 16, 16))

    m["s_cre1"], m["s_cim1"] = LC_(inp["s5_c_re"][0]), LC_(inp["s5_c_im"][0])
    m["s_d"] = np.ascontiguousarray(inp["s5_d"][0].reshape(4, 128).T)
    m["w_glu"] = inp["w_glu"][0]
    m["w_out"] = inp["w_out"][0]
    m["ln1_g"], m["ln1_b"] = inp["ln1_g"], inp["ln1_b"]
    m["ln2_g"], m["ln2_b"] = inp["ln2_g"], inp["ln2_b"]
    m["peer_wq"] = inp["peer_wq"][0]
    m["keysT"] = np.ascontiguousarray(inp["peer_keys"][0].reshape(16, 128, 128).transpose(2, 0, 1))
    m["peer_uv"] = np.concatenate([inp["peer_u"][0], inp["peer_v"][0]], axis=1)
    m.update(host_consts())
    return {k: np.ascontiguousarray(v, dtype=np.float32) for k, v in m.items()}


IN_SHAPES = {
    "x2": [NB, L, D], "ctx2": [NB, LC, D], "ccT": [128, 8, 3], "w_mod": [D, 6144], "b_mod": [1, 6144],
    "w_in": [D, 3264], "qg": [128, 3], "kvg": [128, 2], "w_uq": [384, 1536], "w_ukv": [256, 2048],
    "s_are1": [128, 2, 16], "s_aim1": [128, 2, 16], "s_ldt1": [128, 2, 16],
    "s_are2": [128, 2, 4, 64], "s_aim2": [128, 2, 4, 64], "s_ldt2": [128, 2, 4, 64],
    "s_bre2": [128, 2, 4, 64], "s_bim2": [128, 2, 4, 64],
    "s_cre1": [128, 2, 16, 16], "s_cim1": [128, 2, 16, 16], "s_d": [128, 4],
    "w_glu": [512, 2048], "w_out": [D, D], "ln1_g": [1, D], "ln1_b": [1, D], "ln2_g": [1, D], "ln2_b": [1, D],
    "peer_wq": [D, 2048], "keysT": [128, 16, 128], "peer_uv": [16384, 2 * D],
    "ident": [128, 128], "ropecos": [64, L], "ropesin": [64, L], "ropeRT": [64, 64], "ones": [128, 128],
    "iota_s": [128, T], "iota_c": [128, NCH], "cmask": [128, LT], "pmask": [128, 4], "iota16": [128, 2048],
}

SCR_SHAPES = {
    "modrows": [3, 6144],
    "cqT": [NB, 384, L], "ckvT": [NB, 256, LT], "kpeT": [NB, 64, LT], "uT": [NB, 512, LT],
    "gm": [NB, L, D], "gs": [NB, L, D], "att": [NB, L, D], "gyT": [NB, 512, L], "h1": [NB, L, D],
}


class Ctx:
    _u = 0

    def uniq(self, n):
        Ctx._u += 1
        return f"{n}_{Ctx._u}"


def build(phases=("p0", "p1", "p2", "p3", "p4", "p5"), ext_in=(), ext_out=(), nb=NB, ntile=16):
    nc = bass.Bass("TRN2", target_bir_lowering=False)
    K = Ctx()
    K.nc = nc
    K.nb = nb
    K.ntile = ntile
    A = {}
    for name, shp in IN_SHAPES.items():
        A[name] = nc.dram_tensor(name, shp, F32, kind="ExternalInput").ap()
    for name, shp in SCR_SHAPES.items():
        kind = "ExternalInput" if name in ext_in else ("ExternalOutput" if name in ext_out else "Internal")
        A[name] = nc.dram_tensor(name, shp, F32, kind=kind).ap()
    A["out"] = nc.dram_tensor("out", [NB, L, D], F32, kind="ExternalOutput").ap()
    K.A = A
    with ExitStack() as st:
        P = Prog(nc, st)
        K.P = P
        K.psum = st.enter_context(nc.psum_tensor("psum_all", [128, 4096], F32))
        K.ident = st.enter_context(nc.sbuf_tensor("sb_ident", [128, 128], F32))
        P.dma("sync", K.ident[:], A["ident"], writes=["ident"])
        K.rr = 0
        if "p0" in phases:
            phase0(K)
        if "p1" in phases:
            phase1(K)
        if "p2" in phases:
            phase2(K)
        if "p3" in phases:
            phase3(K)
        if "p4" in phases:
            phase4(K)
        if "p5" in phases:
            phase5(K)
        P.barrier()
        P.flush()
    K.ninst = P.ninst
    return nc, K


def bank(K, i):
    return K.psum[:, i * 512:(i + 1) * 512]


def evac(K, out, in_, reads, writes):
    K.rr += 1
    if K.rr % 2 == 0:
        K.P.op("scalar", lambda e: e.copy(out=out, in_=in_), reads=reads, writes=writes)
    else:
        K.P.op("vector", lambda e: e.tensor_copy(out=out, in_=in_), reads=reads, writes=writes)


def end_phase(K):
    K.P.barrier()
    K.P.flush()


def layernorm_stats(K, ph_sb, xt, xkey, tag):
    P = K.P
    st_, mv, rs = ph_sb["st"], ph_sb["mv"], ph_sb["rs"]
    k = lambda n: tag + n
    P.op("vector", lambda e: e.bn_stats(out=st_[:, 0:6], in_=xt[:, 0:512]), reads=[xkey], writes=[k("st0")])
    P.op("vector", lambda e: e.bn_stats(out=st_[:, 6:12], in_=xt[:, 512:1024]), reads=[xkey], writes=[k("st1")])
    P.op("vector", lambda e: e.bn_aggr(out=mv[:, 0:2], in_=st_[:, 0:12]), reads=[k("st0"), k("st1")], writes=[k("mv")])
    P.op("vector", lambda e: e.tensor_scalar(out=rs[:, 0:1], in0=mv[:, 1:2], scalar1=EPS, scalar2=None, op0=ALU.add), reads=[k("mv")], writes=[k("rs0")])
    P.op("scalar", lambda e: e.activation(out=rs[:, 1:2], in_=rs[:, 0:1], func=AF.Sqrt), reads=[k("rs0")], writes=[k("rs1")])
    P.op("vector", lambda e: e.reciprocal(out=rs[:, 2:3], in_=rs[:, 1:2]), reads=[k("rs1")], writes=[k("rs2")])
    return mv[:, 0:1], rs[:, 2:3]


def ln_bufs(K, ph):
    nc = K.nc
    return {
        "st": ph.enter_context(nc.sbuf_tensor(K.uniq("ln_st"), [128, 12], F32)),
        "mv": ph.enter_context(nc.sbuf_tensor(K.uniq("ln_mv"), [128, 2], F32)),
        "rs": ph.enter_context(nc.sbuf_tensor(K.uniq("ln_rs"), [128, 4], F32)),
    }


def phase0(K):
    nc, P, A = K.nc, K.P, K.A
    with ExitStack() as ph:
        sb = lambda n, s, dt=F32: ph.enter_context(nc.sbuf_tensor(f"q1_{n}", s, dt))
        ccs = sb("ccs", [128, 8, 3])
        bm3 = sb("bm3", [3, 6144])
        mrow = sb("mrow", [3, 6144])
        wm = [sb("wm0", [128, 8, 512]), sb("wm1", [128, 8, 512])]
        P.dma("sync", ccs[:], A["ccT"], writes=["ccs"])
        P.dma("scalar", bm3[:], A["b_mod"].to_broadcast([3, 6144]), writes=["bm3"])
        P.op("scalar", lambda e: e.activation(out=ccs[:], in_=ccs[:], func=AF.Silu), reads=["ccs"], writes=["ccs"])
        wv = A["w_mod"].rearrange("(kc p) n -> p kc n", p=128)
        for nt in range(12):
            w = wm[nt % 2]
            wk = f"wm{nt % 2}"
            P.dma("sync" if nt % 2 == 0 else "scalar", w[:], wv[:, :, nt * 512:(nt + 1) * 512], writes=[wk])
            pb = bank(K, nt % 2)
            pk = f"ps{nt % 2}"
            for kc in range(8):
                P.op("tensor", lambda e, kc=kc, w=w, pb=pb: e.matmul(pb[0:3, :], lhsT=ccs[:, kc, :], rhs=w[:, kc, :], start=(kc == 0), stop=(kc == 7)),
                     reads=["ccs", wk], writes=[pk])
            P.op("vector", lambda e, nt=nt, pb=pb: e.tensor_tensor(out=mrow[:, nt * 512:(nt + 1) * 512], in0=pb[0:3, :], in1=bm3[:, nt * 512:(nt + 1) * 512], op=ALU.add),
                 reads=[pk, "bm3"], writes=["mrow"])
        P.dma("sync", A["modrows"], mrow[:], reads=["mrow"], writes=["d_modrows"])
        end_phase(K)


def load_modT(K, sb, name):
    P, A = K.P, K.A
    t = sb(name, [128, 3, 48])
    for r in range(3):
        for q in range(4):
            src = A["modrows"][r, q * 1536:(q + 1) * 1536].rearrange("(j p) -> p j", p=128)
            P.dma(("sync", "scalar")[(r * 4 + q) % 2], t[:, r, q * 12:(q + 1) * 12], src, reads=["d_modrows"], writes=[name],
                  allow_slow_non_contiguous=True)
    return t


def phase1(K):
    nc, P, A = K.nc, K.P, K.A
    with ExitStack() as ph:
        sb = lambda n, s, dt=F32: ph.enter_context(nc.sbuf_tensor(f"q2_{n}", s, dt))
        win = sb("win", [128, 8, 3264])
        for kc in range(8):
            P.dma(("sync", "scalar")[kc % 2], win[:, kc, :], A["w_in"][kc * 128:(kc + 1) * 128, :], writes=["win"])
        modT = load_modT(K, sb, "modT")
        ops1 = sb("ops1", [128, 3, 8])
        P.op("vector", lambda e: e.tensor_scalar(out=ops1[:], in0=modT[:, :, 8:16], scalar1=1.0, scalar2=None, op0=ALU.add), reads=["modT"], writes=["ops1"])
        lnb = ln_bufs(K, ph)
        xt = [sb("xt0", [128, D]), sb("xt1", [128, D])]
        xn = sb("xn", [128, D])
        xT = [sb("xT0", [128, 8, 512]), sb("xT1", [128, 8, 512])]
        stg = [sb("stg0", [128, 512]), sb("stg1", [128, 512])]
        stm = [sb("stm0", [128, 2048]), sb("stm1", [128, 2048])]
        cnt = {"ti": 0, "si": 0}
        qitems = [(b, kind, t0, nt) for b in range(K.nb) for (kind, t0, nt) in ([("ctx", 0, 2)] + [("lat", q * 4, 4) for q in range(4)])]

        def stage_A(qn):
            b, kind, t0, nt = qitems[qn]
            r = 2 if kind == "ctx" else b
            xq = xT[qn % 2]
            xqk = f"xT{qn % 2}"
            for tl in range(nt):
                x_t = xt[cnt["ti"] % 2]
                xk = f"xt{cnt['ti'] % 2}"
                cnt["ti"] += 1
                src = A["ctx2"][b, (t0 + tl) * 128:(t0 + tl + 1) * 128, :] if kind == "ctx" else A["x2"][b, (t0 + tl) * 128:(t0 + tl + 1) * 128, :]
                P.dma("sync", x_t[:], src, writes=[xk])
                mean, rstd = layernorm_stats(K, lnb, x_t, xk, "p1")
                P.op("vector", lambda e, x_t=x_t, mean=mean, rstd=rstd: e.tensor_scalar(out=xn[:], in0=x_t[:], scalar1=mean, scalar2=rstd, op0=ALU.subtract, op1=ALU.mult),
                     reads=[xk, "p1mv", "p1rs2"], writes=["xn"])
                for half in range(2):
                    pb = bank(K, half)
                    pk = f"ps{half}"
                    for j in range(4):
                        kc = half * 4 + j
                        P.op("tensor", lambda e, kc=kc, j=j, pb=pb: e.transpose(out=pb[:, j * 128:(j + 1) * 128], in_=xn[:, kc * 128:(kc + 1) * 128], identity=K.ident[:]),
                             reads=["xn", "ident"], writes=[pk])
                    for j in range(4):
                        kc = half * 4 + j
                        P.op("scalar", lambda e, kc=kc, j=j, pb=pb, xq=xq, tl=tl, r=r: e.activation(
                            out=xq[:, kc, tl * 128:(tl + 1) * 128], in_=pb[:, j * 128:(j + 1) * 128], func=AF.Identity,
                            scale=ops1[:, r, kc:kc + 1], bias=modT[:, r, kc:kc + 1]), reads=[pk, "ops1", "modT"], writes=[xqk])

        def stage_B(qn):
            b, kind, t0, nt = qitems[qn]
            xq = xT[qn % 2]
            xqk = f"xT{qn % 2}"
            ntok = nt * 128
            tokoff = (0 if kind == "ctx" else LC) + t0 * 128
            secs = []
            if kind == "lat":
                secs += [("cqT", 0 + j * 128, 128, 128, j * 128, t0 * 128) for j in range(3)]
            secs += [("ckvT", 384 + j * 128, 128, 128, j * 128, tokoff) for j in range(2)]
            secs += [("kpeT", 640, 128, 64, 0, tokoff)]
            secs += [("uT", 704 + j * 128, 128, 128, j * 128, tokoff) for j in range(4)]
            for (dn, c0, M, Mo, r0, toff) in secs:
                si = cnt["si"]
                cnt["si"] += 1
                bi = 2 + (si % 4)
                pb = bank(K, bi)
                pk = f"ps{bi}"
                s_ = stg[si % 2]
                sk = f"stg{si % 2}"
                for kc in range(8):
                    P.op("tensor", lambda e, kc=kc, c0=c0, M=M, pb=pb, xq=xq, ntok=ntok: e.matmul(pb[0:M, 0:ntok], lhsT=win[:, kc, c0:c0 + M], rhs=xq[:, kc, 0:ntok], start=(kc == 0), stop=(kc == 7)),
                         reads=["win", xqk], writes=[pk])
                evac(K, s_[0:Mo, 0:ntok], pb[0:Mo, 0:ntok], [pk], [sk])
                P.dma("gpsimd" if si % 2 else "scalar", A[dn][b, r0:r0 + Mo, toff:toff + ntok], s_[0:Mo, 0:ntok], reads=[sk], writes=["d_" + dn])
            if kind == "lat":
                for tl in range(4):
                    s_ = stm[tl % 2]
                    sk = f"stm{tl % 2}"
                    for ncn in range(4):
                        si = cnt["si"]
                        cnt["si"] += 1
                        bi = 2 + (si % 4)
                        pb = bank(K, bi)
                        pk = f"ps{bi}"
                        for kc in range(8):
                            P.op("tensor", lambda e, kc=kc, pb=pb, xq=xq, tl=tl, ncn=ncn: e.matmul(pb[:, :], lhsT=xq[:, kc, tl * 128:(tl + 1) * 128], rhs=win[:, kc, 1216 + ncn * 512:1216 + (ncn + 1) * 512], start=(kc == 0), stop=(kc == 7)),
                                 reads=["win", xqk], writes=[pk])
                        evac(K, s_[:, ncn * 512:(ncn + 1) * 512], pb[:, :], [pk], [sk + f"_{ncn}"])
                    rows = slice((t0 + tl) * 128, (t0 + tl + 1) * 128)
                    P.dma("gpsimd", A["gm"][b, rows, :], s_[:, 0:1024], reads=[sk + "_0", sk + "_1"], writes=["d_gm"])
                    P.dma("scalar", A["gs"][b, rows, :], s_[:, 1024:2048], reads=[sk + "_2", sk + "_3"], writes=["d_gs"])

        stage_A(0)
        for qn in range(len(qitems)):
            if qn + 1 < len(qitems):
                stage_A(qn + 1)
            stage_B(qn)
        end_phase(K)


def phase2(K):
    nc, P, A = K.nc, K.P, K.A
    SC = 192.0 ** -0.5
    SK = ["ps0", "ps1", "ps2", "ps3", "ps4"]
    with ExitStack() as ph:
        sb = lambda n, s, dt=F32: ph.enter_context(nc.sbuf_tensor(f"q3_{n}", s, dt))
        wuq = sb("wuq", [128, 3, 1536])
        wukv = sb("wukv", [128, 2, 2048])
        P.dma("sync", wuq[:], A["w_uq"].rearrange("(kc p) n -> p kc n", p=128), writes=["wuq"])
        P.dma("scalar", wukv[:], A["w_ukv"].rearrange("(kc p) n -> p kc n", p=128), writes=["wukv"])
        qg = sb("qg", [128, 3]); kvg = sb("kvg", [128, 2])
        P.dma("sync", qg[:], A["qg"], writes=["qg"]); P.dma("sync", kvg[:], A["kvg"], writes=["kvg"])
        ones = sb("ones", [128, 128]); P.dma("sync", ones[:], A["ones"], writes=["ones"])
        rcos = sb("rcos", [128, L]); rsin = sb("rsin", [128, L]); rRT = sb("rRT", [128, 128])
        for hh in range(2):
            P.dma("sync", rcos[hh * 64:(hh + 1) * 64, :], A["ropecos"], writes=["rcos"]); P.dma("scalar", rsin[hh * 64:(hh + 1) * 64, :], A["ropesin"], writes=["rsin"])
        P.op("gpsimd", lambda e: e.memset(rRT[:], 0.0), writes=["rRT"])
        P.dma("sync", rRT[0:64, 0:64], A["ropeRT"], writes=["rRT"])
        wpe = sb("wpe", [128, 3, 8, 128])
        P.op("gpsimd", lambda e: e.memset(wpe[:], 0.0), writes=["wpe"])
        for kc in range(3):
            P.dma(("sync", "scalar")[kc % 2], wpe[:, kc, :, 0:64], A["w_uq"][kc * 128:(kc + 1) * 128, :].rearrange("p (h c) -> p h c", c=192)[:, :, 128:192], writes=["wpe"])
        cq = sb("cq", [128, 3, L]); ckv = sb("ckv", [128, 2, LT]); kpe = sb("kpe", [128, LT])
        P.op("gpsimd", lambda e: e.memset(kpe[:], 0.0), writes=["kpe"])
        sq = sb("sq", [128, 512]); rstd = sb("rstd", [128, 512])
        KT = sb("KT", [128, LT]); V = sb("V", [128, NCH, 128])
        QN = sb("QN", [128, L]); QP = sb("QP", [128, L])
        xq = sb("xq", [128, 512]); t1 = sb("t1", [128, 512]); t2 = sb("t2", [128, 512])
        Pm = [sb(f"Pm{i}", [128, LT]) for i in range(2)]; PT = [sb(f"PT{i}", [128, NCH, 128]) for i in range(2)]
        atth = sb("atth", [128, 16, 128])
        sm = sb("sm", [128, 16])
        for b in range(K.nb):
            P.dma("sync", cq[:], A["cqT"][b].rearrange("(j p) n -> p j n", p=128), reads=["d_cqT"], writes=["cq"])
            P.dma("scalar", ckv[:], A["ckvT"][b].rearrange("(j p) n -> p j n", p=128), reads=["d_ckvT"], writes=["ckv"])
            P.dma("sync", kpe[0:64, :], A["kpeT"][b], reads=["d_kpeT"], writes=["kpe"])
            for (buf, bk, nj, ntok, gv, gk) in ((cq, "cq", 3, L, qg, "qg"), (ckv, "ckv", 2, LT, kvg, "kvg")):
                for c0 in range(0, ntok, 512):
                    n = min(512, ntok - c0)
                    pb = bank(K, 0)
                    for j in range(nj):
                        P.op("scalar", lambda e, j=j, c0=c0, n=n, buf=buf: e.activation(out=sq[:, 0:n], in_=buf[:, j, c0:c0 + n], func=AF.Square), reads=[bk], writes=["sq"])
                        P.op("tensor", lambda e, j=j, n=n, pb=pb, nj=nj: e.matmul(pb[:, 0:n], lhsT=ones[:, :], rhs=sq[:, 0:n], start=(j == 0), stop=(j == nj - 1)), reads=["sq", "ones"], writes=["ps0"])
                    P.op("vector", lambda e, n=n, pb=pb, nj=nj: e.tensor_scalar(out=rstd[:, 0:n], in0=pb[:, 0:n], scalar1=1.0 / (nj * 128), scalar2=EPS, op0=ALU.mult, op1=ALU.add), reads=["ps0"], writes=["rstd"])
                    P.op("scalar", lambda e, n=n: e.activation(out=rstd[:, 0:n], in_=rstd[:, 0:n], func=AF.Sqrt), reads=["rstd"], writes=["rstd"])
                    P.op("vector", lambda e, n=n: e.reciprocal(out=rstd[:, 0:n], in_=rstd[:, 0:n]), reads=["rstd"], writes=["rstd"])
                    for j in range(nj):
                        P.op("vector", lambda e, j=j, c0=c0, n=n, buf=buf, gv=gv: e.scalar_tensor_tensor(out=buf[:, j, c0:c0 + n], in0=buf[:, j, c0:c0 + n], scalar=gv[:, j:j + 1], in1=rstd[:, 0:n], op0=ALU.mult, op1=ALU.mult),
                             reads=[bk, gk, "rstd"], writes=[bk])
            for c0 in range(0, L, 512):
                pb = bank(K, 1)
                P.op("tensor", lambda e, c0=c0, pb=pb: e.matmul(pb[:, :], lhsT=rRT[:, :], rhs=kpe[:, LC + c0:LC + c0 + 512], start=True, stop=True), reads=["kpe", "rRT"], writes=["ps1"])
                P.op("vector", lambda e, c0=c0: e.tensor_tensor(out=t1[:], in0=kpe[:, LC + c0:LC + c0 + 512], in1=rcos[:, c0:c0 + 512], op=ALU.mult), reads=["kpe", "rcos"], writes=["t1"])
                P.op("vector", lambda e, c0=c0, pb=pb: e.tensor_tensor(out=t2[:], in0=pb[:, :], in1=rsin[:, c0:c0 + 512], op=ALU.mult), reads=["ps1", "rsin"], writes=["t2"])
                P.op("vector", lambda e, c0=c0: e.tensor_tensor(out=kpe[:, LC + c0:LC + c0 + 512], in0=t1[:], in1=t2[:], op=ALU.add), reads=["t1", "t2"], writes=["kpe"])
            for h in range(8):
                for ci, c0 in enumerate(range(0, LT, 512)):
                    n = min(512, LT - c0)
                    bi = 5 + ci % 3
                    pb = bank(K, bi)
                    for kc in range(2):
                        P.op("tensor", lambda e, kc=kc, c0=c0, n=n, pb=pb, h=h: e.matmul(pb[:, 0:n], lhsT=wukv[:, kc, h * 256:h * 256 + 128], rhs=ckv[:, kc, c0:c0 + n], start=(kc == 0), stop=(kc == 1)),
                             reads=["wukv", "ckv"], writes=[f"ps{bi}"])
                    evac(K, KT[:, c0:c0 + n], pb[:, 0:n], [f"ps{bi}"], ["KT"])
                for g4 in range(0, NCH, 4):
                    ng = min(4, NCH - g4)
                    bi = 5 + (g4 // 4) % 3
                    pb = bank(K, bi)
                    for tt in range(ng):
                        for kc in range(2):
                            P.op("tensor", lambda e, kc=kc, tt=tt, g4=g4, pb=pb, h=h: e.matmul(pb[:, tt * 128:(tt + 1) * 128], lhsT=ckv[:, kc, (g4 + tt) * 128:(g4 + tt + 1) * 128], rhs=wukv[:, kc, h * 256 + 128:h * 256 + 256], start=(kc == 0), stop=(kc == 1)),
                                 reads=["wukv", "ckv"], writes=[f"ps{bi}"])
                    evac(K, V[:, g4:g4 + ng, :], pb[:, 0:ng * 128], [f"ps{bi}"], ["V"])
                for ci, c0 in enumerate(range(0, L, 512)):
                    bi = 5 + ci % 3
                    pb = bank(K, bi)
                    for kc in range(3):
                        P.op("tensor", lambda e, kc=kc, c0=c0, pb=pb, h=h: e.matmul(pb[:, :], lhsT=wuq[:, kc, h * 192:h * 192 + 128], rhs=cq[:, kc, c0:c0 + 512], start=(kc == 0), stop=(kc == 2)),
                             reads=["wuq", "cq"], writes=[f"ps{bi}"])
                    P.op("scalar", lambda e, c0=c0, pb=pb: e.mul(out=QN[:, c0:c0 + 512], in_=pb[:, :], mul=SC), reads=[f"ps{bi}"], writes=["QN"])
                    bi2 = 5 + (ci + 1) % 3
                    pb2 = bank(K, bi2)
                    for kc in range(3):
                        P.op("tensor", lambda e, kc=kc, c0=c0, pb2=pb2, h=h: e.matmul(pb2[:, :], lhsT=wpe[:, kc, h, :], rhs=cq[:, kc, c0:c0 + 512], start=(kc == 0), stop=(kc == 2)),
                             reads=["wpe", "cq"], writes=[f"ps{bi2}"])
                    P.op("scalar", lambda e, pb2=pb2: e.mul(out=xq[:], in_=pb2[:, :], mul=SC), reads=[f"ps{bi2}"], writes=["xq"])
                    bi3 = 5 + (ci + 2) % 3
                    pb3 = bank(K, bi3)
                    P.op("tensor", lambda e, pb3=pb3: e.matmul(pb3[:, :], lhsT=rRT[:, :], rhs=xq[:, :], start=True, stop=True), reads=["xq", "rRT"], writes=[f"ps{bi3}"])
                    P.op("vector", lambda e, c0=c0: e.tensor_tensor(out=t1[:], in0=xq[:], in1=rcos[:, c0:c0 + 512], op=ALU.mult), reads=["xq", "rcos"], writes=["t1"])
                    P.op("vector", lambda e, c0=c0, pb3=pb3: e.tensor_tensor(out=t2[:], in0=pb3[:, :], in1=rsin[:, c0:c0 + 512], op=ALU.mult), reads=[f"ps{bi3}", "rsin"], writes=["t2"])
                    P.op("vector", lambda e, c0=c0: e.tensor_tensor(out=QP[:, c0:c0 + 512], in0=t1[:], in1=t2[:], op=ALU.add), reads=["t1", "t2"], writes=["QP"])
                def emit_S(qt):
                    qs = slice(qt * 128, (qt + 1) * 128)
                    for ci, c0 in enumerate(range(0, LT, 512)):
                        n = min(512, LT - c0)
                        pb = bank(K, ci)
                        P.op("tensor", lambda e, c0=c0, n=n, pb=pb, qs=qs: e.matmul(pb[:, 0:n], lhsT=QN[:, qs], rhs=KT[:, c0:c0 + n], start=True, stop=False), reads=["QN", "KT"], writes=[f"ps{ci}"])
                    for ci, c0 in enumerate(range(0, LT, 512)):
                        n = min(512, LT - c0)
                        pb = bank(K, ci)
                        P.op("tensor", lambda e, c0=c0, n=n, pb=pb, qs=qs: e.matmul(pb[:, 0:n], lhsT=QP[:, qs], rhs=kpe[:, c0:c0 + n], start=False, stop=True), reads=["QP", "kpe"], writes=[f"ps{ci}"])

                S = K.psum[:, 0:LT]

                def softmax_a(qt):
                    o = (qt % 4) * 4
                    pmb = Pm[qt % 2]
                    P.op("vector", lambda e, o=o: e.tensor_reduce(out=sm[:, o:o + 1], in_=S, axis=AX.X, op=ALU.max, negate=True), reads=SK, writes=[f"sm{o}"])
                    P.op("scalar", lambda e, o=o, pmb=pmb: e.activation(out=pmb[:, :], in_=S, func=AF.Exp, bias=sm[:, o:o + 1], scale=1.0, accum_out=sm[:, o + 1:o + 2]), reads=SK + [f"sm{o}"], writes=[f"Pm{qt % 2}", f"sm{o + 1}"])

                def softmax_b(qt):
                    o = (qt % 4) * 4
                    P.op("vector", lambda e, o=o: e.reciprocal(out=sm[:, o + 2:o + 3], in_=sm[:, o + 1:o + 2]), reads=[f"sm{o + 1}"], writes=[f"sm{o + 2}"])

                def emit_T(qt):
                    pmb = Pm[qt % 2]
                    ptb = PT[qt % 2]
                    for g4 in range(0, NCH, 4):
                        ng = min(4, NCH - g4)
                        bi = 5 + (g4 // 4) % 2
                        pb = bank(K, bi)
                        for tt in range(ng):
                            P.op("tensor", lambda e, tt=tt, g4=g4, pb=pb, pmb=pmb: e.transpose(out=pb[:, tt * 128:(tt + 1) * 128], in_=pmb[:, (g4 + tt) * 128:(g4 + tt + 1) * 128], identity=K.ident[:]),
                                 reads=[f"Pm{qt % 2}", "ident"], writes=[f"ps{bi}"])
                        evac(K, ptb[:, g4:g4 + ng, :], pb[:, 0:ng * 128], [f"ps{bi}"], [f"PT{qt % 2}"])

                def emit_PV(qt):
                    o = (qt % 4) * 4
                    ptb = PT[qt % 2]
                    pb = bank(K, 7)
                    for kt in range(NCH):
                        P.op("tensor", lambda e, kt=kt, pb=pb, ptb=ptb: e.matmul(pb[:, 0:128], lhsT=ptb[:, kt, :], rhs=V[:, kt, :], start=(kt == 0), stop=(kt == NCH - 1)), reads=[f"PT{qt % 2}", "V"], writes=["ps7"])
                    P.op("scalar", lambda e, qt=qt, pb=pb, o=o: e.activation(out=atth[:, qt, :], in_=pb[:, 0:128], func=AF.Identity, scale=sm[:, o + 2:o + 3]), reads=["ps7", f"sm{o + 2}"], writes=["atth"])

                emit_S(0)
                softmax_a(0)
                softmax_b(0)
                for qt in range(16):
                    if qt + 1 < 16:
                        emit_S(qt + 1)
                        softmax_a(qt + 1)
                    if qt >= 1:
                        emit_PV(qt - 1)
                    emit_T(qt)
                    if qt + 1 < 16:
                        softmax_b(qt + 1)
                emit_PV(15)
                dst = A["att"][b].rearrange("(qt p) (hh d) -> p qt hh d", p=128, d=128)[:, :, h, :]
                P.dma("gpsimd", dst, atth[:], reads=["atth"], writes=["d_att"])
        end_phase(K)


def reduce_turns(K, w, n, tv, ti, tf, tm, key, shift=0.0):
    P = K.P
    P.op("vector", lambda e: e.tensor_scalar(out=tv[:, 0:n], in0=w, scalar1=float(shift), scalar2=None, op0=ALU.add), reads=[key], writes=["tv"])
    P.op("vector", lambda e: e.tensor_copy(out=ti[:, 0:n], in_=tv[:, 0:n]), reads=["tv"], writes=["ti"])
    P.op("vector", lambda e: e.tensor_copy(out=tf[:, 0:n], in_=ti[:, 0:n]), reads=["ti"], writes=["tf"])
    P.op("vector", lambda e: e.tensor_tensor(out=tv[:, 0:n], in0=tv[:, 0:n], in1=tf[:, 0:n], op=ALU.subtract), reads=["tv", "tf"], writes=["tv"])
    P.op("vector", lambda e: e.tensor_scalar(out=tm[:, 0:n], in0=tv[:, 0:n], scalar1=0.5, scalar2=None, op0=ALU.is_gt), reads=["tv"], writes=["tm"])
    P.op("vector", lambda e: e.tensor_tensor(out=tv[:, 0:n], in0=tv[:, 0:n], in1=tm[:, 0:n], op=ALU.subtract), reads=["tv", "tm"], writes=["tv"])
    P.op("vector", lambda e: e.tensor_scalar(out=tm[:, 0:n], in0=tv[:, 0:n], scalar1=-0.5, scalar2=None, op0=ALU.is_lt), reads=["tv"], writes=["tm"])
    P.op("vector", lambda e: e.tensor_tensor(out=tv[:, 0:n], in0=tv[:, 0:n], in1=tm[:, 0:n], op=ALU.add), reads=["tv", "tm"], writes=["tv"])


def sincos_turns(K, w, n, out_cos, out_sin, tmps, key, okeys):
    tv, ti, tf, tm = tmps
    P = K.P
    reduce_turns(K, w, n, tv, ti, tf, tm, key, 0.0)
    P.op("scalar", lambda e: e.activation(out=out_sin, in_=tv[:, 0:n], func=AF.Sin, scale=TWO_PI), reads=["tv"], writes=[okeys[1]])
    reduce_turns(K, w, n, tv, ti, tf, tm, key, 0.25)
    P.op("scalar", lambda e: e.activation(out=out_cos, in_=tv[:, 0:n], func=AF.Sin, scale=TWO_PI), reads=["tv"], writes=[okeys[0]])


def accurate_exp(K, x, n, out, tmps, xkey, okey):
    P = K.P
    tv, ti, tf, tm = tmps
    LN2_HI, LN2_LO = 0.693145751953125, 1.428606765330187e-06
    V = lambda f_, reads, writes: P.op("vector", f_, reads=reads, writes=writes)
    V(lambda e: e.tensor_scalar(out=tv[:, 0:n], in0=x, scalar1=1.0 / math.log(2.0), scalar2=None, op0=ALU.mult), [xkey], ["tv"])
    V(lambda e: e.tensor_copy(out=ti[:, 0:n], in_=tv[:, 0:n]), ["tv"], ["ti"])
    V(lambda e: e.tensor_copy(out=tf[:, 0:n], in_=ti[:, 0:n]), ["ti"], ["tf"])
    V(lambda e: e.scalar_tensor_tensor(out=tv[:, 0:n], in0=tf[:, 0:n], scalar=-LN2_HI, in1=x, op0=ALU.mult, op1=ALU.add), ["tf", xkey], ["tv"])
    V(lambda e: e.scalar_tensor_tensor(out=tv[:, 0:n], in0=tf[:, 0:n], scalar=-LN2_LO, in1=tv[:, 0:n], op0=ALU.mult, op1=ALU.add), ["tf", "tv"], ["tv"])
    V(lambda e: e.memset(tm[:, 0:n], 1.0), [], ["tm"])
    for d_ in range(12, 0, -1):
        V(lambda e: e.tensor_tensor(out=tm[:, 0:n], in0=tm[:, 0:n], in1=tv[:, 0:n], op=ALU.mult), ["tm", "tv"], ["tm"])
        V(lambda e, d_=d_: e.tensor_scalar(out=tm[:, 0:n], in0=tm[:, 0:n], scalar1=1.0 / d_, scalar2=1.0, op0=ALU.mult, op1=ALU.add), ["tm"], ["tm"])
    V(lambda e: e.tensor_scalar(out=tf[:, 0:n], in0=tf[:, 0:n], scalar1=127.0, scalar2=None, op0=ALU.add), ["tf"], ["tf"])
    V(lambda e: e.tensor_copy(out=ti[:, 0:n], in_=tf[:, 0:n]), ["tf"], ["ti"])
    V(lambda e: e.tensor_single_scalar(out=ti[:, 0:n], in_=ti[:, 0:n], scalar=23, op=ALU.logical_shift_left), ["ti"], ["ti"])
    V(lambda e: e.tensor_tensor(out=out, in0=tm[:, 0:n], in1=ti[:, 0:n].bitcast(F32), op=ALU.mult), ["tm", "ti"], [okey])


def phase3(K):
    nc, P, A = K.nc, K.P, K.A
    INV2PI = 1.0 / (2.0 * math.pi)
    with ExitStack() as ph:
        sb = lambda n, s, dt=F32: ph.enter_context(nc.sbuf_tensor(f"q7_{n}", s, dt))
        cs = sb("cs", [128, 32, T]); sn = sb("sn", [128, 32, T])
        E3c = sb("E3c", [128, 32, NCH]); E3s = sb("E3s", [128, 32, NCH]); E4c = sb("E4c", [128, 32, NCH]); E4s = sb("E4s", [128, 32, NCH])
        coefT = sb("coefT", [128, 32, NCH])
        rr_ = sb("r", [128, 32])
        BW = sb("BW", [128, 2, 2, 4, 128])
        cre1 = sb("cre1", [128, 2, 16, 16]); cimn = sb("cimn", [128, 2, 16, 16])
        stg = [[sb(f"stg{p_}_{ri}", [128, 128]) for ri in range(2)] for p_ in range(4)]
        cmask = sb("cmask", [128, LT]); pmask = sb("pmask", [128, 4]); sd = sb("sd", [128, 4])
        P.dma("sync", cmask[:], A["cmask"], writes=["cmask"]); P.dma("sync", pmask[:], A["pmask"], writes=["pmask"]); P.dma("sync", sd[:], A["s_d"], writes=["sd"])
        P.dma("sync", cre1[:], A["s_cre1"], writes=["cre1"]); P.dma("scalar", cimn[:], A["s_cim1"], writes=["cimn"])
        P.op("vector", lambda e: e.tensor_scalar(out=cimn[:], in0=cimn[:], scalar1=-1.0, scalar2=None, op0=ALU.mult), reads=["cimn"], writes=["cimn"])
        for p_ in range(4):
            for ri in range(2):
                P.op("gpsimd", lambda e, p_=p_, ri=ri: e.memset(stg[p_][ri][:], 0.0), writes=[f"stg{p_}_{ri}"])
        with ExitStack() as ts:
            tb = lambda n, s, dt=F32: ts.enter_context(nc.sbuf_tensor(f"q3t_{n}", s, dt))
            tv = tb("tv", [128, 4096]); ti = tb("ti", [128, 4096], I32); tf = tb("tf", [128, 4096]); tm = tb("tm", [128, 4096])
            tmps = (tv, ti, tf, tm)
            wbig = tb("wbig", [128, 4096])
            iota_s = tb("iota_s", [128, T]); iota_c = tb("iota_c", [128, NCH])
            P.dma("sync", iota_s[:], A["iota_s"], writes=["iota_s"]); P.dma("sync", iota_c[:], A["iota_c"], writes=["iota_c"])
            a1 = tb("a1", [128, 32]); i1 = tb("i1", [128, 32]); l1 = tb("l1", [128, 32])
            P.dma("sync", a1[:], A["s_are1"].rearrange("p d r -> p (d r)"), writes=["a1"])
            P.dma("scalar", i1[:], A["s_aim1"].rearrange("p d r -> p (d r)"), writes=["i1"])
            P.dma("sync", l1[:], A["s_ldt1"].rearrange("p d r -> p (d r)"), writes=["l1"])
            dt1 = tb("dt1", [128, 32]); ardt = tb("ardt", [128, 32]); w1 = tb("w1", [128, 32]); phiT = tb("phiT", [128, 32]); base3 = tb("base3", [128, 32])
            wc = tb("wc", [128, 32, NCH]); w34 = tb("w34", [128, 32, NCH])
            accurate_exp(K, l1[:], 32, dt1[:], tmps, "l1", "dt1")
            P.op("vector", lambda e: e.tensor_tensor(out=ardt[:], in0=a1[:], in1=dt1[:], op=ALU.mult), reads=["a1", "dt1"], writes=["ardt"])
            P.op("vector", lambda e: e.tensor_tensor(out=w1[:], in0=i1[:], in1=dt1[:], op=ALU.mult), reads=["i1", "dt1"], writes=["w1"])
            P.op("vector", lambda e: e.tensor_scalar(out=w1[:], in0=w1[:], scalar1=INV2PI, scalar2=None, op0=ALU.mult), reads=["w1"], writes=["w1"])
            P.op("scalar", lambda e: e.activation(out=rr_[:], in_=ardt[:], func=AF.Exp), reads=["ardt"], writes=["r"])
            P.op("scalar", lambda e: e.activation(out=l1[:], in_=ardt[:], func=AF.Exp, scale=float(T)), reads=["ardt"], writes=["l1"])
            P.op("vector", lambda e: e.tensor_copy(out=coefT[:], in_=l1[:].unsqueeze(2).to_broadcast([128, 32, NCH])), reads=["l1"], writes=["coefT"])
            P.op("vector", lambda e: e.tensor_tensor(out=wbig[:].rearrange("p (a s) -> p a s", s=T), in0=w1[:].unsqueeze(2).to_broadcast([128, 32, T]),
                                                     in1=iota_s[:].unsqueeze(1).to_broadcast([128, 32, T]), op=ALU.mult), reads=["w1", "iota_s"], writes=["wbig"])
            sincos_turns(K, wbig[:], 4096, cs[:].rearrange("p a s -> p (a s)"), sn[:].rearrange("p a s -> p (a s)"), tmps, "wbig", ("cs", "sn"))
            P.op("vector", lambda e: e.tensor_scalar(out=phiT[:], in0=w1[:], scalar1=float(T), scalar2=None, op0=ALU.mult), reads=["w1"], writes=["phiT"])
            reduce_turns(K, phiT[:], 32, tv, ti, tf, tm, "phiT", 0.0)
            P.op("vector", lambda e: e.tensor_copy(out=phiT[:], in_=tv[:, 0:32]), reads=["tv"], writes=["phiT"])
            P.op("vector", lambda e: e.tensor_tensor(out=base3[:], in0=phiT[:], in1=w1[:], op=ALU.subtract), reads=["phiT", "w1"], writes=["base3"])
            P.op("vector", lambda e: e.tensor_tensor(out=wc[:], in0=phiT[:].unsqueeze(2).to_broadcast([128, 32, NCH]), in1=iota_c[:].unsqueeze(1).to_broadcast([128, 32, NCH]), op=ALU.mult),
                 reads=["phiT", "iota_c"], writes=["wc"])
            P.op("vector", lambda e: e.tensor_tensor(out=w34[:], in0=base3[:].unsqueeze(2).to_broadcast([128, 32, NCH]), in1=wc[:], op=ALU.subtract), reads=["base3", "wc"], writes=["w34"])
            sincos_turns(K, w34[:].rearrange("p a c -> p (a c)"), 32 * NCH, E3c[:].rearrange("p a c -> p (a c)"), E3s[:].rearrange("p a c -> p (a c)"), tmps, "w34", ("E3c", "E3s"))
            P.op("vector", lambda e: e.tensor_tensor(out=w34[:], in0=w1[:].unsqueeze(2).to_broadcast([128, 32, NCH]), in1=wc[:], op=ALU.add), reads=["w1", "wc"], writes=["w34"])
            sincos_turns(K, w34[:].rearrange("p a c -> p (a c)"), 32 * NCH, E4c[:].rearrange("p a c -> p (a c)"), E4s[:].rearrange("p a c -> p (a c)"), tmps, "w34", ("E4c", "E4s"))
            for (tb_, k_) in ((E4c, "E4c"), (E4s, "E4s")):
                P.op("vector", lambda e, tb_=tb_: e.tensor_tensor(out=tb_[:], in0=tb_[:], in1=rr_[:].unsqueeze(2).to_broadcast([128, 32, NCH]), op=ALU.mult), reads=[k_, "r"], writes=[k_])
            a2 = tb("a2", [128, 512]); i2 = tb("i2", [128, 512]); l2 = tb("l2", [128, 512]); br2 = tb("br2", [128, 512]); bi2 = tb("bi2", [128, 512])
            for (t_, nm, q_) in ((a2, "s_are2", "sync"), (i2, "s_aim2", "scalar"), (l2, "s_ldt2", "sync"), (br2, "s_bre2", "scalar"), (bi2, "s_bim2", "sync")):
                P.dma(q_, t_[:], A[nm].rearrange("p d j q -> p (d j q)"), writes=[nm])
            x1 = tb("x1", [128, 512]); x2 = tb("x2", [128, 512]); x3 = tb("x3", [128, 512]); x4 = tb("x4", [128, 512]); x5 = tb("x5", [128, 512]); x6 = tb("x6", [128, 512])
            V_ = lambda f_, **kw: P.op("vector", f_, **kw)
            accurate_exp(K, l2[:], 512, l2[:], tmps, "s_ldt2", "s_ldt2")
            V_(lambda e: e.tensor_tensor(out=x1[:], in0=a2[:], in1=l2[:], op=ALU.mult), reads=["s_are2", "s_ldt2"], writes=["x1"])
            V_(lambda e: e.tensor_tensor(out=x2[:], in0=i2[:], in1=l2[:], op=ALU.mult), reads=["s_aim2", "s_ldt2"], writes=["x2"])
            V_(lambda e: e.tensor_scalar(out=x2[:], in0=x2[:], scalar1=INV2PI, scalar2=None, op0=ALU.mult), reads=["x2"], writes=["x2"])
            P.op("scalar", lambda e: e.activation(out=x1[:], in_=x1[:], func=AF.Exp), reads=["x1"], writes=["x1"])
            sincos_turns(K, x2[:], 512, x3[:], x4[:], tmps, "x2", ("x3", "x4"))
            V_(lambda e: e.tensor_tensor(out=x3[:], in0=x3[:], in1=x1[:], op=ALU.mult), reads=["x3", "x1"], writes=["x3"])
            V_(lambda e: e.tensor_tensor(out=x4[:], in0=x4[:], in1=x1[:], op=ALU.mult), reads=["x4", "x1"], writes=["x4"])
            V_(lambda e: e.tensor_scalar(out=x3[:], in0=x3[:], scalar1=-1.0, scalar2=None, op0=ALU.add), reads=["x3"], writes=["x3"])
            V_(lambda e: e.tensor_tensor(out=x1[:], in0=a2[:], in1=a2[:], op=ALU.mult), reads=["s_are2"], writes=["x1"])
            V_(lambda e: e.tensor_tensor(out=x2[:], in0=i2[:], in1=i2[:], op=ALU.mult), reads=["s_aim2"], writes=["x2"])
            V_(lambda e: e.tensor_tensor(out=x1[:], in0=x1[:], in1=x2[:], op=ALU.add), reads=["x1", "x2"], writes=["x1"])
            V_(lambda e: e.reciprocal(out=x1[:], in_=x1[:]), reads=["x1"], writes=["x1"])
            V_(lambda e: e.tensor_tensor(out=x5[:], in0=x3[:], in1=a2[:], op=ALU.mult), reads=["x3", "s_are2"], writes=["x5"])
            V_(lambda e: e.tensor_tensor(out=x6[:], in0=x4[:], in1=i2[:], op=ALU.mult), reads=["x4", "s_aim2"], writes=["x6"])
            V_(lambda e: e.tensor_tensor(out=x5[:], in0=x5[:], in1=x6[:], op=ALU.add), reads=["x5", "x6"], writes=["x5"])
            V_(lambda e: e.tensor_tensor(out=x5[:], in0=x5[:], in1=x1[:], op=ALU.mult), reads=["x5", "x1"], writes=["x5"])
            V_(lambda e: e.tensor_tensor(out=x6[:], in0=x4[:], in1=a2[:], op=ALU.mult), reads=["x4", "s_are2"], writes=["x6"])
            V_(lambda e: e.tensor_tensor(out=x2[:], in0=x3[:], in1=i2[:], op=ALU.mult), reads=["x3", "s_aim2"], writes=["x2"])
            V_(lambda e: e.tensor_tensor(out=x6[:], in0=x6[:], in1=x2[:], op=ALU.subtract), reads=["x6", "x2"], writes=["x6"])
            V_(lambda e: e.tensor_tensor(out=x6[:], in0=x6[:], in1=x1[:], op=ALU.mult), reads=["x6", "x1"], writes=["x6"])
            V_(lambda e: e.tensor_tensor(out=x3[:], in0=x5[:], in1=br2[:], op=ALU.mult), reads=["x5", "s_bre2"], writes=["x3"])
            V_(lambda e: e.tensor_tensor(out=x2[:], in0=x6[:], in1=bi2[:], op=ALU.mult), reads=["x6", "s_bim2"], writes=["x2"])
            V_(lambda e: e.tensor_tensor(out=x3[:], in0=x3[:], in1=x2[:], op=ALU.subtract), reads=["x3", "x2"], writes=["x3"])
            V_(lambda e: e.tensor_tensor(out=x4[:], in0=x5[:], in1=bi2[:], op=ALU.mult), reads=["x5", "s_bim2"], writes=["x4"])
            V_(lambda e: e.tensor_tensor(out=x2[:], in0=x6[:], in1=br2[:], op=ALU.mult), reads=["x6", "s_bre2"], writes=["x2"])
            V_(lambda e: e.tensor_tensor(out=x4[:], in0=x4[:], in1=x2[:], op=ALU.add), reads=["x4", "x2"], writes=["x4"])
            for ri, (src, sk) in enumerate(((x3, "x3"), (x4, "x4"))):
                for g2 in range(2):
                    V_(lambda e, ri=ri, g2=g2, src=src: e.tensor_scalar(out=BW[:, :, ri, :, g2 * 64:(g2 + 1) * 64], in0=src[:].rearrange("p (d j q) -> p d j q", d=2, j=4),
                                                                       scalar1=pmask[:, g2:g2 + 1], scalar2=None, op0=ALU.mult), reads=[sk, "pmask"], writes=["BW"])
            P.barrier()
            P.flush()
        coef = [sb(f"coef{i}", [128, LT]) for i in range(2)]
        uf = sb("uf", [128, LT]); ur = sb("ur", [128, LT])
        btr = sb("btr", [128, LT]); bti = sb("bti", [128, LT])
        gr = [sb(f"gr{i}", [128, LT]) for i in range(2)]
        gi = [sb(f"gi{i}", [128, LT]) for i in range(2)]
        hr = sb("hr", [128, L]); hi = sb("hi", [128, L])
        t1 = sb("t1", [128, 512]); t2 = sb("t2", [128, 512]); t3 = sb("t3", [128, L])
        ytot = sb("ytot", [128, L])
        bx = sb("bx", [128, 8, NCH])
        YK = ["ps4", "ps5", "ps6", "ps7"]
        itb = [0]
        items = [(b, j, d, pos) for b in range(K.nb) for j in range(4) for d in range(2) for pos in range(4)]

        def stage_A(n):
            b, j, d, pos = items[n]
            p2 = n % 2
            if d == 0 and pos == 0:
                P.dma("sync", uf[:], A["uT"][b, j * 128:(j + 1) * 128, :], reads=["d_uT"], writes=["uf"])
                P.op("gpsimd", lambda e: e.tensor_copy(out=ur[:, 0:LC], in_=uf[:, LC - 1::-1]), reads=["uf"], writes=["ur"])
                P.op("gpsimd", lambda e: e.tensor_copy(out=ur[:, LC:LT], in_=uf[:, LT - 1:LC - 1:-1]), reads=["uf"], writes=["ur"])
            u = uf if d == 0 else ur
            uk = "uf" if d == 0 else "ur"
            rb = j * 4 + pos
            a_ = d * 16 + rb
            for ri, csrc in enumerate((cre1, cimn)):
                for g2 in range(2):
                    P.op("gpsimd", lambda e, ri=ri, g2=g2, csrc=csrc, pos=pos, d=d, rb=rb: e.tensor_scalar(
                        out=stg[pos][ri][:, pos * 32 + g2 * 16:pos * 32 + (g2 + 1) * 16], in0=csrc[:, d, rb, :], scalar1=pmask[:, 2 + g2:3 + g2], scalar2=None, op0=ALU.mult),
                        reads=["cre1", "cimn", "pmask"], writes=[f"stg{pos}_{ri}"])
            P.op("scalar", lambda e, a_=a_, p2=p2: e.activation(out=coef[p2][:], in_=cmask[:], func=AF.Identity, scale=rr_[:, a_:a_ + 1]), reads=["cmask", "r"], writes=[f"coef{p2}"])
            for ci, c0 in enumerate(range(0, LT, 512)):
                n_ = min(512, LT - c0)
                nk = n_ // 128
                b0_ = (itb[0] % 2) * 2
                itb[0] += 1
                pre, pim = bank(K, b0_), bank(K, b0_ + 1)
                kre, kim = f"ps{b0_}", f"ps{b0_ + 1}"
                rows = slice(pos * 32, (pos + 1) * 32)
                P.op("tensor", lambda e, pre=pre, n_=n_, rows=rows, d=d, j=j, u=u, c0=c0, pos=pos: e.matmul(pre[:, 0:n_], lhsT=BW[rows, d, 0, j, :], rhs=u[rows, c0:c0 + n_], start=True, stop=True, tile_position=(pos * 32, 0)),
                     reads=["BW", uk], writes=[kre])
                P.op("tensor", lambda e, pim=pim, n_=n_, rows=rows, d=d, j=j, u=u, c0=c0, pos=pos: e.matmul(pim[:, 0:n_], lhsT=BW[rows, d, 1, j, :], rhs=u[rows, c0:c0 + n_], start=True, stop=True, tile_position=(pos * 32, 0)),
                     reads=["BW", uk], writes=[kim])
                v3 = lambda ap_: ap_.rearrange("p (c s) -> p c s", s=T)
                csb = cs[:, a_:a_ + 1, :].to_broadcast([128, nk, T])
                snb = sn[:, a_:a_ + 1, :].to_broadcast([128, nk, T])
                obr, obi = btr[:, c0:c0 + n_], bti[:, c0:c0 + n_]
                P.op("vector", lambda e, csb=csb, pre=pre, n_=n_, v3=v3, obr=obr: e.tensor_tensor(out=v3(obr), in0=csb, in1=v3(pre[:, 0:n_]), op=ALU.mult), reads=["cs", kre], writes=["btr"])
                P.op("vector", lambda e, snb=snb, pim=pim, n_=n_, v3=v3: e.tensor_tensor(out=v3(t1[:, 0:n_]), in0=snb, in1=v3(pim[:, 0:n_]), op=ALU.mult), reads=["sn", kim], writes=["t1"])
                P.op("vector", lambda e, csb=csb, pim=pim, n_=n_, v3=v3, obi=obi: e.tensor_tensor(out=v3(obi), in0=csb, in1=v3(pim[:, 0:n_]), op=ALU.mult), reads=["cs", kim], writes=["bti"])
                P.op("vector", lambda e, snb=snb, pre=pre, n_=n_, v3=v3: e.tensor_tensor(out=v3(t2[:, 0:n_]), in0=snb, in1=v3(pre[:, 0:n_]), op=ALU.mult), reads=["sn", kre], writes=["t2"])
                P.op("vector", lambda e, n_=n_, obr=obr: e.tensor_tensor(out=obr, in0=obr, in1=t1[:, 0:n_], op=ALU.add), reads=["btr", "t1"], writes=["btr"])
                P.op("vector", lambda e, n_=n_, obi=obi: e.tensor_tensor(out=obi, in0=obi, in1=t2[:, 0:n_], op=ALU.subtract), reads=["bti", "t2"], writes=["bti"])

        def stage_B(n):
            b, j, d, pos = items[n]
            p2 = n % 2
            rb = j * 4 + pos
            a_ = d * 16 + rb
            g_r, g_i, cf = gr[p2], gi[p2], coef[p2]
            kgr, kgi, kcf = f"gr{p2}", f"gi{p2}", f"coef{p2}"
            P.op("vector", lambda e: e.tensor_tensor_scan(out=g_r[:], data0=cf[:], data1=btr[:], initial=0.0, op0=ALU.mult, op1=ALU.add), reads=[kcf, "btr"], writes=[kgr])
            P.op("vector", lambda e: e.tensor_tensor_scan(out=g_i[:], data0=cf[:], data1=bti[:], initial=0.0, op0=ALU.mult, op1=ALU.add), reads=[kcf, "bti"], writes=[kgi])
            ger, gei = g_r[:, T - 1:LT:T], g_i[:, T - 1:LT:T]
            e3c, e3s, e4c, e4s, cT = E3c[:, a_, :], E3s[:, a_, :], E4c[:, a_, :], E4s[:, a_, :], coefT[:, a_, :]
            TT = lambda out, in0, in1, op, reads, writes: P.op("vector", lambda e: e.tensor_tensor(out=out, in0=in0, in1=in1, op=op), reads=reads, writes=writes)
            TT(bx[:, 0, :], e3c, ger, ALU.mult, ["E3c", kgr], ["bx0"])
            TT(bx[:, 1, :], e3s, gei, ALU.mult, ["E3s", kgi], ["bx1"])
            TT(bx[:, 2, :], e3c, gei, ALU.mult, ["E3c", kgi], ["bx2"])
            TT(bx[:, 3, :], e3s, ger, ALU.mult, ["E3s", kgr], ["bx3"])
            TT(bx[:, 0, :], bx[:, 0, :], bx[:, 1, :], ALU.subtract, ["bx0", "bx1"], ["bx0"])
            TT(bx[:, 2, :], bx[:, 2, :], bx[:, 3, :], ALU.add, ["bx2", "bx3"], ["bx2"])
            P.op("vector", lambda e: e.tensor_tensor_scan(out=bx[:, 4, :], data0=cT, data1=bx[:, 0, :], initial=0.0, op0=ALU.mult, op1=ALU.add), reads=["coefT", "bx0"], writes=["bx4"])
            P.op("vector", lambda e: e.tensor_tensor_scan(out=bx[:, 5, :], data0=cT, data1=bx[:, 2, :], initial=0.0, op0=ALU.mult, op1=ALU.add), reads=["coefT", "bx2"], writes=["bx5"])
            TT(bx[:, 0, :], e4c, bx[:, 4, :], ALU.mult, ["E4c", "bx4"], ["bx0"])
            TT(bx[:, 1, :], e4s, bx[:, 5, :], ALU.mult, ["E4s", "bx5"], ["bx1"])
            TT(bx[:, 2, :], e4c, bx[:, 5, :], ALU.mult, ["E4c", "bx5"], ["bx2"])
            TT(bx[:, 3, :], e4s, bx[:, 4, :], ALU.mult, ["E4s", "bx4"], ["bx3"])
            TT(bx[:, 6, :], bx[:, 0, :], bx[:, 1, :], ALU.subtract, ["bx0", "bx1"], ["bx6"])
            TT(bx[:, 7, :], bx[:, 2, :], bx[:, 3, :], ALU.add, ["bx2", "bx3"], ["bx7"])
            TT(btr[:, T:LT:T], btr[:, T:LT:T], bx[:, 6, 0:NCH - 1], ALU.add, ["btr", "bx6"], ["btr"])
            TT(bti[:, T:LT:T], bti[:, T:LT:T], bx[:, 7, 0:NCH - 1], ALU.add, ["bti", "bx7"], ["bti"])
            P.op("vector", lambda e: e.tensor_tensor_scan(out=g_r[:, LC:LT], data0=cf[:, LC:LT], data1=btr[:, LC:LT], initial=0.0, op0=ALU.mult, op1=ALU.add), reads=[kcf, "btr"], writes=[kgr])
            P.op("vector", lambda e: e.tensor_tensor_scan(out=g_i[:, LC:LT], data0=cf[:, LC:LT], data1=bti[:, LC:LT], initial=0.0, op0=ALU.mult, op1=ALU.add), reads=[kcf, "bti"], writes=[kgi])

        def stage_C(n):
            b, j, d, pos = items[n]
            p2 = n % 2
            rb = j * 4 + pos
            a_ = d * 16 + rb
            g_r, g_i = gr[p2], gi[p2]
            kgr, kgi = f"gr{p2}", f"gi{p2}"
            v3l = lambda ap_: ap_.rearrange("p (c s) -> p c s", s=T)
            csl = cs[:, a_:a_ + 1, :].to_broadcast([128, 16, T])
            snl = sn[:, a_:a_ + 1, :].to_broadcast([128, 16, T])
            G_ = lambda out, in0, in1, op, reads, writes: P.op("vector", lambda e: e.tensor_tensor(out=out, in0=in0, in1=in1, op=op), reads=reads, writes=writes)
            G_(v3l(hr[:]), v3l(g_r[:, LC:LT]), csl, ALU.mult, ["cs", kgr], ["hr"])
            G_(v3l(t3[:]), v3l(g_i[:, LC:LT]), snl, ALU.mult, ["sn", kgi], ["t3"])
            G_(hr[:], hr[:], t3[:], ALU.subtract, ["hr", "t3"], ["hr"])
            G_(v3l(hi[:]), v3l(g_i[:, LC:LT]), csl, ALU.mult, ["cs", kgi], ["hi"])
            G_(v3l(t3[:]), v3l(g_r[:, LC:LT]), snl, ALU.mult, ["sn", kgr], ["t3"])
            G_(hi[:], hi[:], t3[:], ALU.add, ["hi", "t3"], ["hi"])

        def stage_D(n):
            b, j, d, pos = items[n]
            for nt in range(4):
                yb = bank(K, 4 + nt)
                P.op("tensor", lambda e, yb=yb, nt=nt, pos=pos: e.matmul(yb[:, :], lhsT=stg[pos][0][:, :], rhs=hr[:, nt * 512:(nt + 1) * 512], start=(pos == 0), stop=False),
                     reads=[f"stg{pos}_0", "hr"], writes=[YK[nt]])
                P.op("tensor", lambda e, yb=yb, nt=nt, pos=pos: e.matmul(yb[:, :], lhsT=stg[pos][1][:, :], rhs=hi[:, nt * 512:(nt + 1) * 512], start=False, stop=(pos == 3)),
                     reads=[f"stg{pos}_1", "hi"], writes=[YK[nt]])
            if pos == 3:
                Y = K.psum[:, 2048:4096]
                if d == 0:
                    P.op("vector", lambda e, j=j, Y=Y: e.scalar_tensor_tensor(out=ytot[:], in0=uf[:, LC:LT], scalar=sd[:, j:j + 1], in1=Y, op0=ALU.mult, op1=ALU.add), reads=["uf", "sd"] + YK, writes=["ytot"])
                else:
                    P.op("vector", lambda e, Y=Y: e.tensor_tensor(out=ytot[:], in0=ytot[:], in1=Y[:, ::-1], op=ALU.add), reads=["ytot"] + YK, writes=["ytot"])
                    P.op("scalar", lambda e: e.activation(out=ytot[:], in_=ytot[:], func=AF.Gelu_apprx_tanh), reads=["ytot"], writes=["ytot"])
                    P.dma("scalar", A["gyT"][b, j * 128:(j + 1) * 128, :], ytot[:], reads=["ytot"], writes=["d_gyT"])

        NI = len(items)
        stage_A(0)
        for n in range(NI):
            stage_B(n)
            stage_C(n)
            if n + 1 < NI:
                nb_, nj_, nd_, npos_ = items[n + 1]
                if nd_ == 0 and npos_ == 0:
                    stage_D(n)
                    stage_A(n + 1)
                else:
                    stage_A(n + 1)
                    stage_D(n)
            else:
                stage_D(n)
        end_phase(K)


def phase4(K):
    nc, P, A = K.nc, K.P, K.A
    with ExitStack() as ph:
        sb = lambda n, s, dt=F32: ph.enter_context(nc.sbuf_tensor(f"q4_{n}", s, dt))
        wglu = sb("wglu", [128, 4, 2048]); wout = sb("wout", [128, 8, D])
        P.dma("sync", wglu[:], A["w_glu"].rearrange("(kc p) n -> p kc n", p=128), writes=["wglu"])
        P.dma("scalar", wout[:], A["w_out"].rearrange("(kc p) n -> p kc n", p=128), writes=["wout"])
        lg = sb("lg", [128, D]); lb = sb("lb", [128, D]); ga = sb("ga", [128, D])
        P.dma("sync", lg[:], A["ln1_g"].to_broadcast([128, D]), writes=["lg"])
        P.dma("scalar", lb[:], A["ln1_b"].to_broadcast([128, D]), writes=["lb"])
        lnb = ln_bufs(K, ph)
        gy = sb("gy", [128, 4, L])
        att_t = [sb(f"att{i}", [128, D]) for i in range(2)]
        gm_t = [sb(f"gm{i}", [128, D]) for i in range(2)]
        gs_t = [sb(f"gs{i}", [128, D]) for i in range(2)]
        x_t = [sb(f"x{i}", [128, D]) for i in range(2)]
        sgb = [sb(f"sgb{i}", [128, D]) for i in range(2)]; s5o = [sb(f"s5o{i}", [128, D]) for i in range(2)]; mg = [sb(f"mg{i}", [128, D]) for i in range(2)]
        mT = sb("mT", [128, 8, 128])
        tmp = sb("tmp", [128, D]); rt = sb("rt", [128, D]); xn = sb("xn", [128, D])
        items = [(b, t) for b in range(K.nb) for t in range(K.ntile)]

        def stage_A(n):
            b, t = items[n]
            i = n % 2
            rows = slice(t * 128, (t + 1) * 128)
            if t == 0:
                P.dma("sync", gy[:], A["gyT"][b].rearrange("(j p) n -> p j n", p=128), reads=["d_gyT"], writes=["gy"])
            P.dma("sync", att_t[i][:], A["att"][b, rows, :], reads=["d_att"], writes=[f"att{i}"])
            P.dma("scalar", gm_t[i][:], A["gm"][b, rows, :], reads=["d_gm"], writes=[f"gm{i}"])
            P.dma("sync", gs_t[i][:], A["gs"][b, rows, :], reads=["d_gs"], writes=[f"gs{i}"])
            P.dma("scalar", x_t[i][:], A["x2"][b, rows, :], writes=[f"x{i}"])
            for ncn in range(4):
                pb = bank(K, ncn)
                for kc in range(4):
                    P.op("tensor", lambda e, pb=pb, kc=kc, ncn=ncn, rows=rows: e.matmul(pb[:, :], lhsT=gy[:, kc, rows], rhs=wglu[:, kc, ncn * 512:(ncn + 1) * 512], start=(kc == 0), stop=(kc == 3)),
                         reads=["gy", "wglu"], writes=[f"ps{ncn}"])
            P.op("scalar", lambda e: e.activation(out=sgb[i][:], in_=K.psum[:, 1024:2048], func=AF.Sigmoid), reads=["ps2", "ps3"], writes=[f"sgb{i}"])
            P.op("vector", lambda e: e.tensor_tensor(out=s5o[i][:], in0=K.psum[:, 0:1024], in1=sgb[i][:], op=ALU.mult), reads=["ps0", "ps1", f"sgb{i}"], writes=[f"s5o{i}"])
            P.op("scalar", lambda e: e.activation(out=gm_t[i][:], in_=gm_t[i][:], func=AF.Sigmoid), reads=[f"gm{i}"], writes=[f"gm{i}"])
            P.op("scalar", lambda e: e.activation(out=gs_t[i][:], in_=gs_t[i][:], func=AF.Sigmoid), reads=[f"gs{i}"], writes=[f"gs{i}"])
            P.op("gpsimd", lambda e: e.tensor_tensor(out=att_t[i][:], in0=att_t[i][:], in1=gm_t[i][:], op=ALU.mult), reads=[f"att{i}", f"gm{i}"], writes=[f"att{i}"])
            P.op("gpsimd", lambda e: e.tensor_tensor(out=s5o[i][:], in0=s5o[i][:], in1=gs_t[i][:], op=ALU.mult), reads=[f"s5o{i}", f"gs{i}"], writes=[f"s5o{i}"])
            P.op("vector", lambda e: e.tensor_tensor(out=mg[i][:], in0=att_t[i][:], in1=s5o[i][:], op=ALU.add), reads=[f"att{i}", f"s5o{i}"], writes=[f"mg{i}"])

        def stage_B(n):
            b, t = items[n]
            i = n % 2
            rows = slice(t * 128, (t + 1) * 128)
            if t == 0:
                P.dma("scalar", ga[:], A["modrows"][b:b + 1, 2048:3072].to_broadcast([128, D]), reads=["d_modrows"], writes=["ga"])
            for half in range(2):
                pb = bank(K, 4 + half)
                for jj in range(4):
                    kc = half * 4 + jj
                    P.op("tensor", lambda e, pb=pb, jj=jj, kc=kc: e.transpose(out=pb[:, jj * 128:(jj + 1) * 128], in_=mg[i][:, kc * 128:(kc + 1) * 128], identity=K.ident[:]),
                         reads=[f"mg{i}", "ident"], writes=[f"ps{4 + half}"])
                evac(K, mT[:, half * 4:(half + 1) * 4, :], pb[:, :], [f"ps{4 + half}"], ["mT"])
            for ncn in range(2):
                pb = bank(K, 6 + ncn)
                for kc in range(8):
                    P.op("tensor", lambda e, pb=pb, kc=kc, ncn=ncn: e.matmul(pb[:, :], lhsT=mT[:, kc, :], rhs=wout[:, kc, ncn * 512:(ncn + 1) * 512], start=(kc == 0), stop=(kc == 7)),
                         reads=["mT", "wout"], writes=[f"ps{6 + ncn}"])
            P.op("vector", lambda e: e.tensor_tensor(out=tmp[:], in0=K.psum[:, 3072:4096], in1=ga[:], op=ALU.mult), reads=["ps6", "ps7", "ga"], writes=["tmp"])
            P.op("vector", lambda e: e.scalar_tensor_tensor(out=rt[:], in0=x_t[i][:], scalar=float(ALPHA), in1=tmp[:], op0=ALU.mult, op1=ALU.add), reads=[f"x{i}", "tmp"], writes=["rt"])
            mean, rstd = layernorm_stats(K, lnb, rt, "rt", "p4")
            P.op("vector", lambda e: e.tensor_scalar(out=xn[:], in0=rt[:], scalar1=mean, scalar2=rstd, op0=ALU.subtract, op1=ALU.mult), reads=["rt", "p4mv", "p4rs2"], writes=["xn"])
            P.op("gpsimd", lambda e: e.tensor_tensor(out=xn[:], in0=xn[:], in1=lg[:], op=ALU.mult), reads=["xn", "lg"], writes=["xn"])
            P.op("gpsimd", lambda e: e.tensor_tensor(out=xn[:], in0=xn[:], in1=lb[:], op=ALU.add), reads=["xn", "lb"], writes=["xn"])
            P.dma("gpsimd", A["h1"][b, rows, :], xn[:], reads=["xn"], writes=["d_h1"])

        stage_A(0)
        for n in range(len(items)):
            if n + 1 < len(items):
                stage_A(n + 1)
            stage_B(n)
        end_phase(K)


def top16(K, x, xk, wk, sv_ap, si_ap, tag):
    P = K.P
    n = x.shape[1]
    P.op("vector", lambda e: e.max(out=sv_ap[:, 0:8], in_=x), reads=[xk], writes=[tag + "v0"])
    P.op("vector", lambda e: e.max_index(out=si_ap[:, 0:8], in_max=sv_ap[:, 0:8], in_values=x), reads=[xk, tag + "v0"], writes=[tag + "i0"])
    P.op("vector", lambda e: e.match_replace(out=wk[:, 0:n], in_to_replace=sv_ap[:, 0:8], in_values=x, imm_value=-1e30), reads=[xk, tag + "v0"], writes=["cwk"])
    P.op("vector", lambda e: e.max(out=sv_ap[:, 8:16], in_=wk[:, 0:n]), reads=["cwk"], writes=[tag + "v1"])
    P.op("vector", lambda e: e.max_index(out=si_ap[:, 8:16], in_max=sv_ap[:, 8:16], in_values=wk[:, 0:n]), reads=["cwk", tag + "v1"], writes=[tag + "i1"])
    return [tag + "v0", tag + "v1"], [tag + "i0", tag + "i1"]


def phase5(K):
    nc, P, A = K.nc, K.P, K.A
    NBUF = 8
    with ExitStack() as ph:
        sb = lambda n, s, dt=F32: ph.enter_context(nc.sbuf_tensor(f"q5_{n}", s, dt))
        wqq = [sb(f"wqq{i}", [128, 8, 256]) for i in range(2)]
        keysT = sb("keysT", [128, 16, 128]); P.dma("sync", keysT[:], A["keysT"], writes=["keysT"])
        io16 = sb("io16", [128, 16]); P.dma("sync", io16[:], A["iota16"][:, 0:16], writes=["io16"])
        lg = sb("lg", [128, D]); lb = sb("lb", [128, D])
        P.dma("sync", lg[:], A["ln2_g"].to_broadcast([128, D]), writes=["lg"])
        P.dma("scalar", lb[:], A["ln2_b"].to_broadcast([128, D]), writes=["lb"])
        shf = [sb(f"shf{b}", [128, D]) for b in range(K.nb)]
        scf = [sb(f"scf{b}", [128, D]) for b in range(K.nb)]
        gf = [sb(f"gf{b}", [128, D]) for b in range(K.nb)]
        for b in range(K.nb):
            P.dma("sync", shf[b][:], A["modrows"][b:b + 1, 3072:4096].to_broadcast([128, D]), reads=["d_modrows"], writes=[f"shf{b}"])
            P.dma("scalar", scf[b][:], A["modrows"][b:b + 1, 4096:5120].to_broadcast([128, D]), reads=["d_modrows"], writes=[f"scf{b}"])
            P.dma("sync", gf[b][:], A["modrows"][b:b + 1, 5120:6144].to_broadcast([128, D]), reads=["d_modrows"], writes=[f"gf{b}"])
            P.op("vector", lambda e, b=b: e.tensor_scalar(out=scf[b][:], in0=scf[b][:], scalar1=1.0, scalar2=None, op0=ALU.add), reads=[f"scf{b}"], writes=[f"scf{b}"])
        lnb = ln_bufs(K, ph)
        lnb2 = ln_bufs(K, ph)
        h1t = [sb(f"h1t{i}", [128, D]) for i in range(2)]
        xm = [sb(f"xm{i}", [128, D]) for i in range(2)]
        xmT = sb("xmT", [128, 8, 128])
        bufA = sb("bufA", [128, 2048]); bufB = sb("bufB", [128, 2048]); cwk = sb("cwk", [128, 256])
        sv = sb("sv", [128, 16, 16]); si = sb("si", [128, 16, 16], U32); sif = sb("sif", [128, 16, 16])
        tv_ = sb("tv", [128, 8, 16]); tp = sb("tp", [128, 8, 16], U32); tq = sb("tq", [128, 8, 16], U32)
        tif = sb("tif", [128, 128]); tjf = sb("tjf", [128, 128]); e0 = sb("e0", [128, 128]); e1 = sb("e1", [128, 128])
        eidx = [sb(f"eidx{i}", [128, 128], I32) for i in range(2)]
        ex = sb("ex", [128, 8, 16]); ssum = sb("ssum", [128, 16])
        gg = [sb(f"gg{i}", [128, 8, 16]) for i in range(2)]
        act = [sb(f"act{i}", [128, 128]) for i in range(2)]
        wgt = [sb(f"wgt{i}", [128, 128]) for i in range(2)]
        UV = [sb(f"UV{i}", [128, 2 * D]) for i in range(NBUF)]
        prod = [sb(f"prod{i}", [128, D]) for i in range(4)]
        accs = [sb(f"acc{i}", [128, D]) for i in range(4)]; r2 = sb("r2", [128, D])
        wqi = [0]

        def topk_gen(b, t, s):
            rows = slice(t * 128, (t + 1) * 128)
            hk, xk = f"h1t{s}", f"xm{s}"
            P.dma("sync", h1t[s][:], A["h1"][b, rows, :], reads=["d_h1"], writes=[hk])
            mean, rstd = layernorm_stats(K, lnb, h1t[s], hk, "p5")
            P.op("vector", lambda e, mean=mean, rstd=rstd: e.tensor_scalar(out=xm[s][:], in0=h1t[s][:], scalar1=mean, scalar2=rstd, op0=ALU.subtract, op1=ALU.mult), reads=[hk, "p5mv", "p5rs2"], writes=[xk])
            P.op("gpsimd", lambda e: e.tensor_tensor(out=xm[s][:], in0=xm[s][:], in1=scf[b][:], op=ALU.mult), reads=[xk, f"scf{b}"], writes=[xk])
            P.op("gpsimd", lambda e: e.tensor_tensor(out=xm[s][:], in0=xm[s][:], in1=shf[b][:], op=ALU.add), reads=[xk, f"shf{b}"], writes=[xk])
            yield
            for half in range(2):
                pb = bank(K, half)
                for jj in range(4):
                    kc = half * 4 + jj
                    P.op("tensor", lambda e, pb=pb, jj=jj, kc=kc: e.transpose(out=pb[:, jj * 128:(jj + 1) * 128], in_=xm[s][:, kc * 128:(kc + 1) * 128], identity=K.ident[:]),
                         reads=[xk, "ident"], writes=[f"ps{half}"])
                evac(K, xmT[:, half * 4:(half + 1) * 4, :], pb[:, :], [f"ps{half}"], ["xmT"])
            yield
            qT = bufA[:].rearrange("p (a s) -> p a s", s=128)
            for g4 in range(4):
                bi = 2 + g4 % 2
                pb = bank(K, bi)
                for h2 in range(2):
                    w = wqq[wqi[0] % 2]
                    wk_ = f"wqq{wqi[0] % 2}"
                    wqi[0] += 1
                    cb = g4 * 512 + h2 * 256
                    P.dma("scalar", w[:], A["peer_wq"].rearrange("(kc p) n -> p kc n", p=128)[:, :, cb:cb + 256], writes=[wk_])
                    for j2 in range(2):
                        jj = h2 * 2 + j2
                        for kc in range(8):
                            P.op("tensor", lambda e, pb=pb, jj=jj, j2=j2, kc=kc, w=w: e.matmul(pb[:, jj * 128:(jj + 1) * 128], lhsT=w[:, kc, j2 * 128:(j2 + 1) * 128], rhs=xmT[:, kc, :], start=(kc == 0), stop=(kc == 7)),
                                 reads=[wk_, "xmT"], writes=[f"ps{bi}"])
                evac(K, bufA[:, g4 * 512:(g4 + 1) * 512], pb[:, :], [f"ps{bi}"], ["bufA"])
                yield
            for g4 in range(4):
                bi = 4 + g4 % 2
                pb = bank(K, bi)
                for jj in range(4):
                    hz = g4 * 4 + jj
                    P.op("tensor", lambda e, pb=pb, jj=jj, hz=hz: e.matmul(pb[:, jj * 128:(jj + 1) * 128], lhsT=qT[:, hz, :], rhs=keysT[:, hz, :], start=True, stop=True),
                         reads=["bufA", "keysT"], writes=[f"ps{bi}"])
                evac(K, bufB[:, g4 * 512:(g4 + 1) * 512], pb[:, :], [f"ps{bi}"], ["bufB"])
            yield
            svk, sik = [], []
            for hz in range(16):
                a_, b_ = top16(K, bufB[:, hz * 128:(hz + 1) * 128], "bufB", cwk, sv[:, hz, :], si[:, hz, :], f"s1_{hz}_")
                svk += a_
                sik += b_
                if hz % 2 == 1:
                    yield
            P.op("vector", lambda e: e.tensor_copy(out=sif[:], in_=si[:]), reads=sik, writes=["sif"])
            sv4 = sv[:].rearrange("p (h z) k -> p h z k", z=2)
            sif4 = sif[:].rearrange("p (h z) k -> p h z k", z=2)
            cand4 = bufB[:].rearrange("p (h i j) -> p h i j", h=8, i=16)
            P.op("vector", lambda e: e.tensor_tensor(out=cand4, in0=sv4[:, :, 0, :].unsqueeze(3).to_broadcast([128, 8, 16, 16]), in1=sv4[:, :, 1, :].unsqueeze(2).to_broadcast([128, 8, 16, 16]), op=ALU.add),
                 reads=svk + ["bufB"], writes=["bufB"])
            yield
            tvk, tpk = [], []
            for h in range(8):
                a_, b_ = top16(K, bufB[:, h * 256:(h + 1) * 256], "bufB", cwk, tv_[:, h, :], tp[:, h, :], f"s2_{h}_")
                tvk += a_
                tpk += b_
                if h % 2 == 1:
                    yield
            tp2 = tp[:].rearrange("p h k -> p (h k)")
            tq2 = tq[:].rearrange("p h k -> p (h k)")
            P.op("vector", lambda e: e.tensor_single_scalar(out=tq2, in_=tp2, scalar=4, op=ALU.logical_shift_right), reads=tpk, writes=["tq"])
            P.op("vector", lambda e: e.tensor_copy(out=tif[:], in_=tq2), reads=["tq"], writes=["tif"])
            P.op("vector", lambda e: e.tensor_single_scalar(out=tq2, in_=tp2, scalar=15, op=ALU.bitwise_and), reads=tpk + ["tif"], writes=["tq"])
            P.op("vector", lambda e: e.tensor_copy(out=tjf[:], in_=tq2), reads=["tq"], writes=["tjf"])
            yield
            eq3 = bufA[:].rearrange("p (a i) -> p a i", i=16)
            eq4 = bufA[:].rearrange("p (h k i) -> p h k i", h=8, k=16)
            for (src, sk, z, dst, dk) in ((tif, "tif", 0, e0, "e0"), (tjf, "tjf", 1, e1, "e1")):
                P.op("vector", lambda e, src=src: e.tensor_tensor(out=eq3, in0=src[:].unsqueeze(2).to_broadcast([128, 128, 16]), in1=io16[:].unsqueeze(1).to_broadcast([128, 128, 16]), op=ALU.is_equal),
                     reads=[sk, "io16", "bufA"], writes=["bufA"])
                P.op("vector", lambda e, z=z: e.tensor_tensor(out=eq4, in0=eq4, in1=sif4[:, :, z, :].unsqueeze(2).to_broadcast([128, 8, 16, 16]), op=ALU.mult), reads=["bufA", "sif"], writes=["bufA"])
                P.op("vector", lambda e, dst=dst: e.tensor_reduce(out=dst[:], in_=eq3, axis=AX.X, op=ALU.add), reads=["bufA"], writes=[dk])
                yield
            P.op("vector", lambda e: e.scalar_tensor_tensor(out=e0[:], in0=e0[:], scalar=128.0, in1=e1[:], op0=ALU.mult, op1=ALU.add), reads=["e0", "e1"], writes=["e0"])
            P.op("vector", lambda e: e.tensor_copy(out=eidx[s][:], in_=e0[:]), reads=["e0"], writes=[f"eidx{s}"])
            P.op("vector", lambda e: e.tensor_tensor(out=ex[:], in0=tv_[:], in1=tv_[:, :, 0:1].to_broadcast([128, 8, 16]), op=ALU.subtract), reads=tvk, writes=["ex"])
            P.op("scalar", lambda e: e.activation(out=ex[:], in_=ex[:], func=AF.Exp), reads=["ex"], writes=["ex"])
            P.op("vector", lambda e: e.tensor_reduce(out=ssum[:, 0:8], in_=ex[:], axis=AX.X, op=ALU.add), reads=["ex"], writes=["ssum0"])
            P.op("vector", lambda e: e.reciprocal(out=ssum[:, 8:16], in_=ssum[:, 0:8]), reads=["ssum0"], writes=["ssum1"])
            P.op("vector", lambda e: e.tensor_tensor(out=gg[s][:], in0=ex[:], in1=ssum[:, 8:16].unsqueeze(2).to_broadcast([128, 8, 16]), op=ALU.mult), reads=["ex", "ssum1"], writes=[f"gg{s}"])
            yield

        def drain(g):
            if g is not None:
                for _ in g:
                    pass

        tiles = [(b, t) for b in range(K.nb) for t in range(K.ntile)]
        NTI = len(tiles)
        drain(topk_gen(tiles[0][0], tiles[0][1], 0))
        ring = [0]
        slots = {}
        issued = [0]
        state = {"nxt": None}

        def gather(s, c):
            i = ring[0] % NBUF
            ring[0] += 1
            P.dma_fn("gpsimd", lambda e, i=i, c=c, s=s: e.indirect_dma_start(out=UV[i][:, :], out_offset=None, in_=A["peer_uv"], in_offset=bass.IndirectOffsetOnAxis(ap=eidx[s][:, c:c + 1], axis=0)),
                     reads=[f"eidx{s}"], writes=[f"UV{i}"])
            return i

        def issue_upto(k):
            k = min(k, NTI * 128)
            while issued[0] < k:
                g = issued[0]
                ti_, c_ = divmod(g, 128)
                if c_ == 0 and ti_ > 0:
                    drain(state["nxt"])
                    state["nxt"] = None
                slots[g] = gather(ti_ % 2, c_)
                issued[0] += 1

        def prods(gp):
            ti_, pr = divmod(gp, 64)
            s = ti_ % 2
            c0 = pr * 2
            for kk in range(2):
                c = c0 + kk
                i = slots[ti_ * 128 + c]
                q = (gp % 2) * 2 + kk
                pk = f"prod{q}"
                P.op("vector", lambda e, i=i, q=q, s=s: e.tensor_tensor(out=prod[q][:], in0=UV[i][:, 0:D], in1=xm[s][:], op=ALU.mult), reads=[f"UV{i}", f"xm{s}"], writes=[pk])
                P.op("scalar", lambda e, q=q, c=c, s=s: e.activation(out=prod[q][:], in_=prod[q][:], func=AF.Identity, accum_out=act[s][:, c:c + 1]), reads=[pk], writes=[pk, f"actp{q}"])
            q0 = (gp % 2) * 2
            P.op("scalar", lambda e, c0=c0, s=s: e.activation(out=wgt[s][:, c0:c0 + 2], in_=act[s][:, c0:c0 + 2], func=AF.Gelu_apprx_tanh), reads=[f"actp{q0}", f"actp{q0 + 1}"], writes=[f"wgtp{gp % 2}"])

        def finish(gp):
            ti_, pr = divmod(gp, 64)
            s = ti_ % 2
            c0 = pr * 2
            gg2 = gg[s][:].rearrange("p h k -> p (h k)")
            P.op("vector", lambda e, c0=c0, s=s, gg2=gg2: e.tensor_tensor(out=wgt[s][:, c0:c0 + 2], in0=wgt[s][:, c0:c0 + 2], in1=gg2[:, c0:c0 + 2], op=ALU.mult), reads=[f"wgtp{gp % 2}", f"gg{s}"], writes=[f"wgtq{gp % 2}"])
            for kk in range(2):
                c = c0 + kk
                i = slots[ti_ * 128 + c]
                ak = f"acc{s}_{kk}"
                a_t = accs[s * 2 + kk]
                P.op("vector", lambda e, i=i, c=c, s=s, a_t=a_t: e.scalar_tensor_tensor(out=a_t[:], in0=UV[i][:, D:2 * D], scalar=wgt[s][:, c:c + 1], in1=a_t[:], op0=ALU.mult, op1=ALU.add),
                     reads=[f"UV{i}", f"wgtq{gp % 2}", ak], writes=[ak])
                issue_upto(ti_ * 128 + c + 1 + NBUF)

        def prologue(ti_):
            s = ti_ % 2
            for kk in range(2):
                P.op("gpsimd", lambda e, s=s, kk=kk: e.memset(accs[s * 2 + kk][:], 0.0), writes=[f"acc{s}_{kk}"])
            if ti_ + 1 < NTI:
                state["nxt"] = topk_gen(tiles[ti_ + 1][0], tiles[ti_ + 1][1], 1 - s)

        def epilogue(ti_):
            b, t = tiles[ti_]
            s = ti_ % 2
            rows = slice(t * 128, (t + 1) * 128)
            a0, a1 = accs[s * 2], accs[s * 2 + 1]
            P.op("vector", lambda e: e.tensor_tensor(out=a0[:], in0=a0[:], in1=a1[:], op=ALU.add), reads=[f"acc{s}_0", f"acc{s}_1"], writes=[f"acc{s}_0"])
            P.op("gpsimd", lambda e: e.tensor_tensor(out=r2[:], in0=a0[:], in1=gf[b][:], op=ALU.mult), reads=[f"acc{s}_0", f"gf{b}"], writes=["r2"])
            P.op("vector", lambda e: e.scalar_tensor_tensor(out=r2[:], in0=h1t[s][:], scalar=float(ALPHA), in1=r2[:], op0=ALU.mult, op1=ALU.add), reads=[f"h1t{s}", "r2"], writes=["r2"])
            mean, rstd = layernorm_stats(K, lnb2, r2, "r2", "p5b")
            P.op("vector", lambda e: e.tensor_scalar(out=r2[:], in0=r2[:], scalar1=mean, scalar2=rstd, op0=ALU.subtract, op1=ALU.mult), reads=["r2", "p5bmv", "p5brs2"], writes=["r2"])
            P.op("gpsimd", lambda e: e.tensor_tensor(out=r2[:], in0=r2[:], in1=lg[:], op=ALU.mult), reads=["r2", "lg"], writes=["r2"])
            P.op("gpsimd", lambda e: e.tensor_tensor(out=r2[:], in0=r2[:], in1=lb[:], op=ALU.add), reads=["r2", "lb"], writes=["r2"])
            P.dma("sync", A["out"][b, rows, :], r2[:], reads=["r2"], writes=["d_out"])

        prologue(0)
        issue_upto(NBUF)
        prods(0)
        NGP = NTI * 64
        for gp in range(NGP):
            ti_, pr = divmod(gp, 64)
            if pr == 0 and ti_ > 0:
                prologue(ti_)
            if gp + 1 < NGP:
                prods(gp + 1)
            finish(gp)
            if state["nxt"] is not None and pr % 2 == 1:
                try:
                    next(state["nxt"])
                except StopIteration:
                    state["nxt"] = None
            if pr == 63:
                epilogue(ti_)
        end_phase(K)


_CACHE = {}


def kernel(**inputs):
    inputs = {k: np.asarray(v) for k, v in inputs.items()}
    if "nc" not in _CACHE:
        _CACHE["nc"] = build()[0]
    nc = _CACHE["nc"]
    in_maps = [layout_inputs(inputs, c) for c in range(8)]
    res = run_bass_kernel_spmd(nc, in_maps, core_ids=list(range(8)))
    out = np.concatenate([np.asarray(r["out"]) for r in res.results], axis=0)
    return out.astype(np.float32)
```

```python
import math
from contextlib import ExitStack
import numpy as np
import concourse.bass as bass
import concourse.mybir as mybir
from concourse.bass_utils import run_bass_kernel_spmd

F32 = mybir.dt.float32
I32 = mybir.dt.int32
U32 = mybir.dt.uint32
ALU = mybir.AluOpType
AF = mybir.ActivationFunctionType
AX = mybir.AxisListType

COMPUTE = ("vector", "scalar", "gpsimd", "tensor")
ALLENG = ("sync", "scalar", "gpsimd", "vector", "tensor")
EPOCH = 12000
NDSEM = 24

NB = 2
L = 2048
LC = 256
LT = L + LC
D = 1024
T = 128
NCH = LT // T
EPS = 1e-6
ALPHA = (2.0 * 1) ** 0.25
TWO_PI = 6.283185


class Prog:
    def __init__(self, nc, stack):
        self.nc = nc
        self.stack = stack
        self.ops = {e: [] for e in ALLENG}
        self.csem = {}
        self.ccount = {}
        self.nsem = 0
        for e in COMPUTE:
            self._new_csem(e)
        self.dpool = {}
        for pn, cnt in (("hw", 16), ("sw", NDSEM)):
            self.dpool[pn] = {"sems": [stack.enter_context(nc.semaphore(f"dq{pn}{i}")) for i in range(cnt)], "val": [0] * cnt, "next": 0}
        self.known = {e: {} for e in ALLENG}
        self.last_w = {}
        self.readers = {}
        self.ninst = 0
        self.allsems = {}

    def _new_csem(self, e):
        self.nsem += 1
        self.csem[e] = self.stack.enter_context(self.nc.semaphore(f"c_{e}_{self.nsem}"))
        self.ccount[e] = 0

    def _emit(self, eng, fn, reads, writes, dma):
        deps = {}

        def add(tok, kind):
            if tok is None:
                return
            sem, val, peng, pdma = tok
            if (not pdma) and peng == eng and not dma:
                if eng == "tensor":
                    return
            k = id(sem)
            if k not in deps or deps[k][1] < val:
                deps[k] = (sem, val)

        for r in reads:
            add(self.last_w.get(r), "raw")
        for w in writes:
            add(self.last_w.get(w), "waw")
            for tok in self.readers.get(w, {}).values():
                add(tok, "war")
        if dma:
            pool = self.dpool["sw" if eng == "gpsimd" else "hw"]
            i = pool["next"]
            pool["next"] = (i + 1) % len(pool["sems"])
            if pool["val"][i] > 0:
                k = id(pool["sems"][i])
                if k not in deps or deps[k][1] < pool["val"][i]:
                    deps[k] = (pool["sems"][i], pool["val"][i])
            pool["val"][i] += 16
            tok = (pool["sems"][i], pool["val"][i], eng, True)
            inc = 16
        else:
            if self.ccount[eng] >= EPOCH:
                self._new_csem(eng)
            self.ccount[eng] += 1
            tok = (self.csem[eng], self.ccount[eng], eng, False)
            inc = 1
        self.allsems[id(tok[0])] = (tok[0], tok[1])
        waits = []
        kn = self.known[eng]
        for k, (sem, val) in deps.items():
            if kn.get(k, 0) >= val:
                continue
            kn[k] = val
            waits.append((sem, val))
        tsem = tok[0]

        def run(eh, waits=waits, fn=fn, tsem=tsem, inc=inc):
            for sem, val in waits:
                eh.wait_ge(sem, val)
            fn(eh).then_inc(tsem, inc)

        self.ops[eng].append(run)
        self.ninst += 1
        for w in writes:
            self.last_w[w] = tok
            self.readers[w] = {}
        for r in reads:
            self.readers.setdefault(r, {})[id(tok[0])] = tok
        return tok

    def op(self, eng, fn, reads=(), writes=()):
        return self._emit(eng, fn, list(reads), list(writes), False)

    def dma(self, q, out, in_, reads=(), writes=(), **kw):
        return self._emit(q, lambda e: e.dma_start(out=out, in_=in_, **kw), list(reads), list(writes), True)

    def dma_fn(self, q, fn, reads=(), writes=()):
        return self._emit(q, fn, list(reads), list(writes), True)

    def barrier(self):
        snap = dict(self.allsems)
        for eng in ALLENG:
            waits = []
            kn = self.known[eng]
            for k, (sem, val) in snap.items():
                if kn.get(k, 0) < val:
                    kn[k] = val
                    waits.append((sem, val))

            def run(eh, waits=waits):
                for sem, val in waits:
                    eh.wait_ge(sem, val)

            self.ops[eng].append(run)

    def flush(self):
        nc = self.nc
        ops = self.ops
        with nc.Block() as block:
            @block.sync
            def _(e):
                for f in ops["sync"]:
                    f(e)

            @block.scalar
            def _(e):
                for f in ops["scalar"]:
                    f(e)

            @block.gpsimd
            def _(e):
                for f in ops["gpsimd"]:
                    f(e)

            @block.vector
            def _(e):
                for f in ops["vector"]:
                    f(e)

            @block.tensor
            def _(e):
                for f in ops["tensor"]:
                    f(e)
        self.ops = {e: [] for e in ALLENG}


def host_consts():
    f = np.float32
    c = {}
    c["ident"] = np.eye(128, dtype=f)
    rows = L // 64
    row = np.broadcast_to(np.arange(rows, dtype=f)[:, None], (rows, 64)).reshape(-1)
    col = np.broadcast_to(np.arange(64, dtype=f)[None, :], (rows, 64)).reshape(-1)
    inv_freq = np.power(f(10000.0), (-np.arange(0, 32, 2, dtype=f) / f(32))).astype(f)
    ang_r = (row[:, None] * inv_freq[None, :]).astype(f)
    ang_c = (col[:, None] * inv_freq[None, :]).astype(f)
    cr, sr, cc_, sc_ = np.cos(ang_r).astype(f), np.sin(ang_r).astype(f), np.cos(ang_c).astype(f), np.sin(ang_c).astype(f)
    c["ropecos"] = np.ascontiguousarray(np.concatenate([cr.T, cr.T, cc_.T, cc_.T], 0))
    c["ropesin"] = np.ascontiguousarray(np.concatenate([sr.T, sr.T, sc_.T, sc_.T], 0))
    R = np.zeros((64, 64), f)
    I16 = np.eye(16, dtype=f)
    R[0:16, 16:32] = -I16
    R[16:32, 0:16] = I16
    R[32:48, 48:64] = -I16
    R[48:64, 32:48] = I16
    c["ropeRT"] = np.ascontiguousarray(R.T)
    c["ones"] = np.ones((128, 128), f)
    c["iota_s"] = np.broadcast_to(np.arange(T, dtype=f)[None, :], (128, T)).copy()
    c["iota_c"] = np.broadcast_to(np.arange(NCH, dtype=f)[None, :], (128, NCH)).copy()
    m = np.ones((128, LT), f)
    m[:, ::T] = 0.0
    c["cmask"] = m
    p = np.arange(128)
    c["pmask"] = np.stack([((p // 16) % 2 == 0), ((p // 16) % 2 == 1), (p // 64 == 0), (p // 64 == 1)], 1).astype(f)
    c["iota16"] = np.broadcast_to(np.arange(16, dtype=f)[None, None, :], (128, 128, 16)).reshape(128, 2048).copy()
    return c


def layout_inputs(inp, core):
    f = np.float32
    b0 = core * NB
    m = {}
    m["x2"] = np.ascontiguousarray(inp["x"][b0:b0 + NB])
    m["ctx2"] = np.ascontiguousarray(inp["ctx"][b0:b0 + NB])
    cc = np.concatenate([inp["c"][b0:b0 + NB], inp["c_ctx"][None, :]], 0)
    m["ccT"] = np.ascontiguousarray(cc.reshape(3, 8, 128).transpose(2, 1, 0))
    m["w_mod"] = inp["w_mod"][0]
    m["b_mod"] = inp["b_mod"]
    m["w_in"] = inp["w_in"][0]
    m["qg"] = np.ascontiguousarray(inp["q_norm_g"][0].reshape(3, 128).T)
    m["kvg"] = np.ascontiguousarray(inp["kv_norm_g"][0].reshape(2, 128).T)
    m["w_uq"] = inp["w_uq"][0]
    m["w_ukv"] = inp["w_ukv"][0]

    def L1(a):
        return np.ascontiguousarray(a.reshape(2, 16, 2, 64).transpose(2, 3, 0, 1).reshape(128, 2, 16))

    def L2(a):
        t = a.reshape(2, 4, 8, 64).transpose(2, 0, 1, 3)
        return np.ascontiguousarray(np.broadcast_to(t[:, None], (8, 16, 2, 4, 64)).reshape(128, 2, 4, 64))

    are, aim, ldt = inp["s5_a_re"][0], inp["s5_a_im"][0], inp["s5_log_dt"][0]
    ldtf = np.broadcast_to(ldt[:, :, None], (2, 32, 64))
    m["s_are1"], m["s_aim1"], m["s_ldt1"] = L1(are), L1(aim), L1(ldtf)
    m["s_are2"], m["s_aim2"], m["s_ldt2"] = L2(are), L2(aim), L2(ldtf)

    def LB(b):
        return np.ascontiguousarray(b.reshape(2, 4, 8, 64, 16).transpose(2, 4, 0, 1, 3).reshape(128, 2, 4, 64))

    m["s_bre2"], m["s_bim2"] = LB(inp["s5_b_re"][0]), LB(inp["s5_b_im"][0])

    def LC_(c_):
        return np.ascontiguousarray(c_.reshape(2, 16, 2, 16, 64).transpose(2, 4, 0, 1, 3).reshape(128, 2, 16, 16))

    m["s_cre1"], m["s_cim1"] = LC_(inp["s5_c_re"][0]), LC_(inp["s5_c_im"][0])
    m["s_d"] = np.ascontiguousarray(inp["s5_d"][0].reshape(4, 128).T)
    m["w_glu"] = inp["w_glu"][0]
    m["w_out"] = inp["w_out"][0]
    m["ln1_g"], m["ln1_b"] = inp["ln1_g"], inp["ln1_b"]
    m["ln2_g"], m["ln2_b"] = inp["ln2_g"], inp["ln2_b"]
    m["peer_wq"] = inp["peer_wq"][0]
    m["keysT"] = np.ascontiguousarray(inp["peer_keys"][0].reshape(16, 128, 128).transpose(2, 0, 1))
    m["peer_uv"] = np.concatenate([inp["peer_u"][0], inp["peer_v"][0]], axis=1)
    m.update(host_consts())
    return {k: np.ascontiguousarray(v, dtype=np.float32) for k, v in m.items()}


IN_SHAPES = {
    "x2": [NB, L, D], "ctx2": [NB, LC, D], "ccT": [128, 8, 3], "w_mod": [D, 6144], "b_mod": [1, 6144],
    "w_in": [D, 3264], "qg": [128, 3], "kvg": [128, 2], "w_uq": [384, 1536], "w_ukv": [256, 2048],
    "s_are1": [128, 2, 16], "s_aim1": [128, 2, 16], "s_ldt1": [128, 2, 16],
    "s_are2": [128, 2, 4, 64], "s_aim2": [128, 2, 4, 64], "s_ldt2": [128, 2, 4, 64],
    "s_bre2": [128, 2, 4, 64], "s_bim2": [128, 2, 4, 64],
    "s_cre1": [128, 2, 16, 16], "s_cim1": [128, 2, 16, 16], "s_d": [128, 4],
    "w_glu": [512, 2048], "w_out": [D, D], "ln1_g": [1, D], "ln1_b": [1, D], "ln2_g": [1, D], "ln2_b": [1, D],
    "peer_wq": [D, 2048], "keysT": [128, 16, 128], "peer_uv": [16384, 2 * D],
    "ident": [128, 128], "ropecos": [64, L], "ropesin": [64, L], "ropeRT": [64, 64], "ones": [128, 128],
    "iota_s": [128, T], "iota_c": [128, NCH], "cmask": [128, LT], "pmask": [128, 4], "iota16": [128, 2048],
}

SCR_SHAPES = {
    "modrows": [3, 6144],
    "cqT": [NB, 384, L], "ckvT": [NB, 256, LT], "kpeT": [NB, 64, LT], "uT": [NB, 512, LT],
    "gm": [NB, L, D], "gs": [NB, L, D], "att": [NB, L, D], "gyT": [NB, 512, L], "h1": [NB, L, D],
}


class Ctx:
    _u = 0

    def uniq(self, n):
        Ctx._u += 1
        return f"{n}_{Ctx._u}"


def build(phases=("p0", "p1", "p2", "p3", "p4", "p5"), ext_in=(), ext_out=(), nb=NB, ntile=16):
    nc = bass.Bass("TRN2", target_bir_lowering=False)
    K = Ctx()
    K.nc = nc
    K.nb = nb
    K.ntile = ntile
    A = {}
    for name, shp in IN_SHAPES.items():
        A[name] = nc.dram_tensor(name, shp, F32, kind="ExternalInput").ap()
    for name, shp in SCR_SHAPES.items():
        kind = "ExternalInput" if name in ext_in else ("ExternalOutput" if name in ext_out else "Internal")
        A[name] = nc.dram_tensor(name, shp, F32, kind=kind).ap()
    A["out"] = nc.dram_tensor("out", [NB, L, D], F32, kind="ExternalOutput").ap()
    K.A = A
    with ExitStack() as st:
        P = Prog(nc, st)
        K.P = P
        K.psum = st.enter_context(nc.psum_tensor("psum_all", [128, 4096], F32))
        K.ident = st.enter_context(nc.sbuf_tensor("sb_ident", [128, 128], F32))
        P.dma("sync", K.ident[:], A["ident"], writes=["ident"])
        K.rr = 0
        if "p0" in phases:
            phase0(K)
        if "p1" in phases:
            phase1(K)
        if "p2" in phases:
            phase2(K)
        if "p3" in phases:
            phase3(K)
        if "p4" in phases:
            phase4(K)
        if "p5" in phases:
            phase5(K)
        P.barrier()
        P.flush()
    K.ninst = P.ninst
    return nc, K


def bank(K, i):
    return K.psum[:, i * 512:(i + 1) * 512]


def evac(K, out, in_, reads, writes):
    K.rr += 1
    if K.rr % 2 == 0:
        K.P.op("scalar", lambda e: e.copy(out=out, in_=in_), reads=reads, writes=writes)
    else:
        K.P.op("vector", lambda e: e.tensor_copy(out=out, in_=in_), reads=reads, writes=writes)


def end_phase(K):
    K.P.barrier()
    K.P.flush()


def layernorm_stats(K, ph_sb, xt, xkey, tag):
    P = K.P
    st_, mv, rs = ph_sb["st"], ph_sb["mv"], ph_sb["rs"]
    k = lambda n: tag + n
    P.op("vector", lambda e: e.bn_stats(out=st_[:, 0:6], in_=xt[:, 0:512]), reads=[xkey], writes=[k("st0")])
    P.op("vector", lambda e: e.bn_stats(out=st_[:, 6:12], in_=xt[:, 512:1024]), reads=[xkey], writes=[k("st1")])
    P.op("vector", lambda e: e.bn_aggr(out=mv[:, 0:2], in_=st_[:, 0:12]), reads=[k("st0"), k("st1")], writes=[k("mv")])
    P.op("vector", lambda e: e.tensor_scalar(out=rs[:, 0:1], in0=mv[:, 1:2], scalar1=EPS, scalar2=None, op0=ALU.add), reads=[k("mv")], writes=[k("rs0")])
    P.op("scalar", lambda e: e.activation(out=rs[:, 1:2], in_=rs[:, 0:1], func=AF.Sqrt), reads=[k("rs0")], writes=[k("rs1")])
    P.op("vector", lambda e: e.reciprocal(out=rs[:, 2:3], in_=rs[:, 1:2]), reads=[k("rs1")], writes=[k("rs2")])
    return mv[:, 0:1], rs[:, 2:3]


def ln_bufs(K, ph):
    nc = K.nc
    return {
        "st": ph.enter_context(nc.sbuf_tensor(K.uniq("ln_st"), [128, 12], F32)),
        "mv": ph.enter_context(nc.sbuf_tensor(K.uniq("ln_mv"), [128, 2], F32)),
        "rs": ph.enter_context(nc.sbuf_tensor(K.uniq("ln_rs"), [128, 4], F32)),
    }


def phase0(K):
    nc, P, A = K.nc, K.P, K.A
    with ExitStack() as ph:
        sb = lambda n, s, dt=F32: ph.enter_context(nc.sbuf_tensor(f"q1_{n}", s, dt))
        ccs = sb("ccs", [128, 8, 3])
        bm3 = sb("bm3", [3, 6144])
        mrow = sb("mrow", [3, 6144])
        wm = [sb("wm0", [128, 8, 512]), sb("wm1", [128, 8, 512])]
        P.dma("sync", ccs[:], A["ccT"], writes=["ccs"])
        P.dma("scalar", bm3[:], A["b_mod"].to_broadcast([3, 6144]), writes=["bm3"])
        P.op("scalar", lambda e: e.activation(out=ccs[:], in_=ccs[:], func=AF.Silu), reads=["ccs"], writes=["ccs"])
        wv = A["w_mod"].rearrange("(kc p) n -> p kc n", p=128)
        for nt in range(12):
            w = wm[nt % 2]
            wk = f"wm{nt % 2}"
            P.dma("sync" if nt % 2 == 0 else "scalar", w[:], wv[:, :, nt * 512:(nt + 1) * 512], writes=[wk])
            pb = bank(K, nt % 2)
            pk = f"ps{nt % 2}"
            for kc in range(8):
                P.op("tensor", lambda e, kc=kc, w=w, pb=pb: e.matmul(pb[0:3, :], lhsT=ccs[:, kc, :], rhs=w[:, kc, :], start=(kc == 0), stop=(kc == 7)),
                     reads=["ccs", wk], writes=[pk])
            P.op("vector", lambda e, nt=nt, pb=pb: e.tensor_tensor(out=mrow[:, nt * 512:(nt + 1) * 512], in0=pb[0:3, :], in1=bm3[:, nt * 512:(nt + 1) * 512], op=ALU.add),
                 reads=[pk, "bm3"], writes=["mrow"])
        P.dma("sync", A["modrows"], mrow[:], reads=["mrow"], writes=["d_modrows"])
        end_phase(K)


def load_modT(K, sb, name):
    P, A = K.P, K.A
    t = sb(name, [128, 3, 48])
    for r in range(3):
        for q in range(4):
            src = A["modrows"][r, q * 1536:(q + 1) * 1536].rearrange("(j p) -> p j", p=128)
            P.dma(("sync", "scalar")[(r * 4 + q) % 2], t[:, r, q * 12:(q + 1) * 12], src, reads=["d_modrows"], writes=[name],
                  allow_slow_non_contiguous=True)
    return t


def phase1(K):
    nc, P, A = K.nc, K.P, K.A
    with ExitStack() as ph:
        sb = lambda n, s, dt=F32: ph.enter_context(nc.sbuf_tensor(f"q2_{n}", s, dt))
        win = sb("win", [128, 8, 3264])
        for kc in range(8):
            P.dma(("sync", "scalar")[kc % 2], win[:, kc, :], A["w_in"][kc * 128:(kc + 1) * 128, :], writes=["win"])
        modT = load_modT(K, sb, "modT")
        ops1 = sb("ops1", [128, 3, 8])
        P.op("vector", lambda e: e.tensor_scalar(out=ops1[:], in0=modT[:, :, 8:16], scalar1=1.0, scalar2=None, op0=ALU.add), reads=["modT"], writes=["ops1"])
        lnb = ln_bufs(K, ph)
        xt = [sb("xt0", [128, D]), sb("xt1", [128, D])]
        xn = sb("xn", [128, D])
        xT = [sb("xT0", [128, 8, 512]), sb("xT1", [128, 8, 512])]
        stg = [sb("stg0", [128, 512]), sb("stg1", [128, 512])]
        stm = [sb("stm0", [128, 2048]), sb("stm1", [128, 2048])]
        cnt = {"ti": 0, "si": 0}
        qitems = [(b, kind, t0, nt) for b in range(K.nb) for (kind, t0, nt) in ([("ctx", 0, 2)] + [("lat", q * 4, 4) for q in range(4)])]

        def stage_A(qn):
            b, kind, t0, nt = qitems[qn]
            r = 2 if kind == "ctx" else b
            xq = xT[qn % 2]
            xqk = f"xT{qn % 2}"
            for tl in range(nt):
                x_t = xt[cnt["ti"] % 2]
                xk = f"xt{cnt['ti'] % 2}"
                cnt["ti"] += 1
                src = A["ctx2"][b, (t0 + tl) * 128:(t0 + tl + 1) * 128, :] if kind == "ctx" else A["x2"][b, (t0 + tl) * 128:(t0 + tl + 1) * 128, :]
                P.dma("sync", x_t[:], src, writes=[xk])
                mean, rstd = layernorm_stats(K, lnb, x_t, xk, "p1")
                P.op("vector", lambda e, x_t=x_t, mean=mean, rstd=rstd: e.tensor_scalar(out=xn[:], in0=x_t[:], scalar1=mean, scalar2=rstd, op0=ALU.subtract, op1=ALU.mult),
                     reads=[xk, "p1mv", "p1rs2"], writes=["xn"])
                for half in range(2):
                    pb = bank(K, half)
                    pk = f"ps{half}"
                    for j in range(4):
                        kc = half * 4 + j
                        P.op("tensor", lambda e, kc=kc, j=j, pb=pb: e.transpose(out=pb[:, j * 128:(j + 1) * 128], in_=xn[:, kc * 128:(kc + 1) * 128], identity=K.ident[:]),
                             reads=["xn", "ident"], writes=[pk])
                    for j in range(4):
                        kc = half * 4 + j
                        P.op("scalar", lambda e, kc=kc, j=j, pb=pb, xq=xq, tl=tl, r=r: e.activation(
                            out=xq[:, kc, tl * 128:(tl + 1) * 128], in_=pb[:, j * 128:(j + 1) * 128], func=AF.Identity,
                            scale=ops1[:, r, kc:kc + 1], bias=modT[:, r, kc:kc + 1]), reads=[pk, "ops1", "modT"], writes=[xqk])

        def stage_B(qn):
            b, kind, t0, nt = qitems[qn]
            xq = xT[qn % 2]
            xqk = f"xT{qn % 2}"
            ntok = nt * 128
            tokoff = (0 if kind == "ctx" else LC) + t0 * 128
            secs = []
            if kind == "lat":
                secs += [("cqT", 0 + j * 128, 128, 128, j * 128, t0 * 128) for j in range(3)]
            secs += [("ckvT", 384 + j * 128, 128, 128, j * 128, tokoff) for j in range(2)]
            secs += [("kpeT", 640, 128, 64, 0, tokoff)]
            secs += [("uT", 704 + j * 128, 128, 128, j * 128, tokoff) for j in range(4)]
            for (dn, c0, M, Mo, r0, toff) in secs:
                si = cnt["si"]
                cnt["si"] += 1
                bi = 2 + (si % 4)
                pb = bank(K, bi)
                pk = f"ps{bi}"
                s_ = stg[si % 2]
                sk = f"stg{si % 2}"
                for kc in range(8):
                    P.op("tensor", lambda e, kc=kc, c0=c0, M=M, pb=pb, xq=xq, ntok=ntok: e.matmul(pb[0:M, 0:ntok], lhsT=win[:, kc, c0:c0 + M], rhs=xq[:, kc, 0:ntok], start=(kc == 0), stop=(kc == 7)),
                         reads=["win", xqk], writes=[pk])
                evac(K, s_[0:Mo, 0:ntok], pb[0:Mo, 0:ntok], [pk], [sk])
                P.dma("gpsimd" if si % 2 else "scalar", A[dn][b, r0:r0 + Mo, toff:toff + ntok], s_[0:Mo, 0:ntok], reads=[sk], writes=["d_" + dn])
            if kind == "lat":
                for tl in range(4):
                    s_ = stm[tl % 2]
                    sk = f"stm{tl % 2}"
                    for ncn in range(4):
                        si = cnt["si"]
                        cnt["si"] += 1
                        bi = 2 + (si % 4)
                        pb = bank(K, bi)
                        pk = f"ps{bi}"
                        for kc in range(8):
                            P.op("tensor", lambda e, kc=kc, pb=pb, xq=xq, tl=tl, ncn=ncn: e.matmul(pb[:, :], lhsT=xq[:, kc, tl * 128:(tl + 1) * 128], rhs=win[:, kc, 1216 + ncn * 512:1216 + (ncn + 1) * 512], start=(kc == 0), stop=(kc == 7)),
                                 reads=["win", xqk], writes=[pk])
                        evac(K, s_[:, ncn * 512:(ncn + 1) * 512], pb[:, :], [pk], [sk + f"_{ncn}"])
                    rows = slice((t0 + tl) * 128, (t0 + tl + 1) * 128)
                    P.dma("gpsimd", A["gm"][b, rows, :], s_[:, 0:1024], reads=[sk + "_0", sk + "_1"], writes=["d_gm"])
                    P.dma("scalar", A["gs"][b, rows, :], s_[:, 1024:2048], reads=[sk + "_2", sk + "_3"], writes=["d_gs"])

        stage_A(0)
        for qn in range(len(qitems)):
            if qn + 1 < len(qitems):
                stage_A(qn + 1)
            stage_B(qn)
        end_phase(K)


def phase2(K):
    nc, P, A = K.nc, K.P, K.A
    SC = 192.0 ** -0.5
    SK = ["ps0", "ps1", "ps2", "ps3", "ps4"]
    with ExitStack() as ph:
        sb = lambda n, s, dt=F32: ph.enter_context(nc.sbuf_tensor(f"q3_{n}", s, dt))
        wuq = sb("wuq", [128, 3, 1536])
        wukv = sb("wukv", [128, 2, 2048])
        P.dma("sync", wuq[:], A["w_uq"].rearrange("(kc p) n -> p kc n", p=128), writes=["wuq"])
        P.dma("scalar", wukv[:], A["w_ukv"].rearrange("(kc p) n -> p kc n", p=128), writes=["wukv"])
        qg = sb("qg", [128, 3]); kvg = sb("kvg", [128, 2])
        P.dma("sync", qg[:], A["qg"], writes=["qg"]); P.dma("sync", kvg[:], A["kvg"], writes=["kvg"])
        ones = sb("ones", [128, 128]); P.dma("sync", ones[:], A["ones"], writes=["ones"])
        rcos = sb("rcos", [128, L]); rsin = sb("rsin", [128, L]); rRT = sb("rRT", [128, 128])
        for hh in range(2):
            P.dma("sync", rcos[hh * 64:(hh + 1) * 64, :], A["ropecos"], writes=["rcos"]); P.dma("scalar", rsin[hh * 64:(hh + 1) * 64, :], A["ropesin"], writes=["rsin"])
        P.op("gpsimd", lambda e: e.memset(rRT[:], 0.0), writes=["rRT"])
        P.dma("sync", rRT[0:64, 0:64], A["ropeRT"], writes=["rRT"])
        wpe = sb("wpe", [128, 3, 8, 128])
        P.op("gpsimd", lambda e: e.memset(wpe[:], 0.0), writes=["wpe"])
        for kc in range(3):
            P.dma(("sync", "scalar")[kc % 2], wpe[:, kc, :, 0:64], A["w_uq"][kc * 128:(kc + 1) * 128, :].rearrange("p (h c) -> p h c", c=192)[:, :, 128:192], writes=["wpe"])
        cq = sb("cq", [128, 3, L]); ckv = sb("ckv", [128, 2, LT]); kpe = sb("kpe", [128, LT])
        P.op("gpsimd", lambda e: e.memset(kpe[:], 0.0), writes=["kpe"])
        sq = sb("sq", [128, 512]); rstd = sb("rstd", [128, 512])
        KT = sb("KT", [128, LT]); V = sb("V", [128, NCH, 128])
        QN = sb("QN", [128, L]); QP = sb("QP", [128, L])
        xq = sb("xq", [128, 512]); t1 = sb("t1", [128, 512]); t2 = sb("t2", [128, 512])
        Pm = [sb(f"Pm{i}", [128, LT]) for i in range(2)]; PT = [sb(f"PT{i}", [128, NCH, 128]) for i in range(2)]
        atth = sb("atth", [128, 16, 128])
        sm = sb("sm", [128, 16])
        for b in range(K.nb):
            P.dma("sync", cq[:], A["cqT"][b].rearrange("(j p) n -> p j n", p=128), reads=["d_cqT"], writes=["cq"])
            P.dma("scalar", ckv[:], A["ckvT"][b].rearrange("(j p) n -> p j n", p=128), reads=["d_ckvT"], writes=["ckv"])
            P.dma("sync", kpe[0:64, :], A["kpeT"][b], reads=["d_kpeT"], writes=["kpe"])
            for (buf, bk, nj, ntok, gv, gk) in ((cq, "cq", 3, L, qg, "qg"), (ckv, "ckv", 2, LT, kvg, "kvg")):
                for c0 in range(0, ntok, 512):
                    n = min(512, ntok - c0)
                    pb = bank(K, 0)
                    for j in range(nj):
                        P.op("scalar", lambda e, j=j, c0=c0, n=n, buf=buf: e.activation(out=sq[:, 0:n], in_=buf[:, j, c0:c0 + n], func=AF.Square), reads=[bk], writes=["sq"])
                        P.op("tensor", lambda e, j=j, n=n, pb=pb, nj=nj: e.matmul(pb[:, 0:n], lhsT=ones[:, :], rhs=sq[:, 0:n], start=(j == 0), stop=(j == nj - 1)), reads=["sq", "ones"], writes=["ps0"])
                    P.op("vector", lambda e, n=n, pb=pb, nj=nj: e.tensor_scalar(out=rstd[:, 0:n], in0=pb[:, 0:n], scalar1=1.0 / (nj * 128), scalar2=EPS, op0=ALU.mult, op1=ALU.add), reads=["ps0"], writes=["rstd"])
                    P.op("scalar", lambda e, n=n: e.activation(out=rstd[:, 0:n], in_=rstd[:, 0:n], func=AF.Sqrt), reads=["rstd"], writes=["rstd"])
                    P.op("vector", lambda e, n=n: e.reciprocal(out=rstd[:, 0:n], in_=rstd[:, 0:n]), reads=["rstd"], writes=["rstd"])
                    for j in range(nj):
                        P.op("vector", lambda e, j=j, c0=c0, n=n, buf=buf, gv=gv: e.scalar_tensor_tensor(out=buf[:, j, c0:c0 + n], in0=buf[:, j, c0:c0 + n], scalar=gv[:, j:j + 1], in1=rstd[:, 0:n], op0=ALU.mult, op1=ALU.mult),
                             reads=[bk, gk, "rstd"], writes=[bk])
            for c0 in range(0, L, 512):
                pb = bank(K, 1)
                P.op("tensor", lambda e, c0=c0, pb=pb: e.matmul(pb[:, :], lhsT=rRT[:, :], rhs=kpe[:, LC + c0:LC + c0 + 512], start=True, stop=True), reads=["kpe", "rRT"], writes=["ps1"])
                P.op("vector", lambda e, c0=c0: e.tensor_tensor(out=t1[:], in0=kpe[:, LC + c0:LC + c0 + 512], in1=rcos[:, c0:c0 + 512], op=ALU.mult), reads=["kpe", "rcos"], writes=["t1"])
                P.op("vector", lambda e, c0=c0, pb=pb: e.tensor_tensor(out=t2[:], in0=pb[:, :], in1=rsin[:, c0:c0 + 512], op=ALU.mult), reads=["ps1", "rsin"], writes=["t2"])
                P.op("vector", lambda e, c0=c0: e.tensor_tensor(out=kpe[:, LC + c0:LC + c0 + 512], in0=t1[:], in1=t2[:], op=ALU.add), reads=["t1", "t2"], writes=["kpe"])
            for h in range(8):
                for ci, c0 in enumerate(range(0, LT, 512)):
                    n = min(512, LT - c0)
                    bi = 5 + ci % 3
                    pb = bank(K, bi)
                    for kc in range(2):
                        P.op("tensor", lambda e, kc=kc, c0=c0, n=n, pb=pb, h=h: e.matmul(pb[:, 0:n], lhsT=wukv[:, kc, h * 256:h * 256 + 128], rhs=ckv[:, kc, c0:c0 + n], start=(kc == 0), stop=(kc == 1)),
                             reads=["wukv", "ckv"], writes=[f"ps{bi}"])
                    evac(K, KT[:, c0:c0 + n], pb[:, 0:n], [f"ps{bi}"], ["KT"])
                for g4 in range(0, NCH, 4):
                    ng = min(4, NCH - g4)
                    bi = 5 + (g4 // 4) % 3
                    pb = bank(K, bi)
                    for tt in range(ng):
                        for kc in range(2):
                            P.op("tensor", lambda e, kc=kc, tt=tt, g4=g4, pb=pb, h=h: e.matmul(pb[:, tt * 128:(tt + 1) * 128], lhsT=ckv[:, kc, (g4 + tt) * 128:(g4 + tt + 1) * 128], rhs=wukv[:, kc, h * 256 + 128:h * 256 + 256], start=(kc == 0), stop=(kc == 1)),
                                 reads=["wukv", "ckv"], writes=[f"ps{bi}"])
                    evac(K, V[:, g4:g4 + ng, :], pb[:, 0:ng * 128], [f"ps{bi}"], ["V"])
                for ci, c0 in enumerate(range(0, L, 512)):
                    bi = 5 + ci % 3
                    pb = bank(K, bi)
                    for kc in range(3):
                        P.op("tensor", lambda e, kc=kc, c0=c0, pb=pb, h=h: e.matmul(pb[:, :], lhsT=wuq[:, kc, h * 192:h * 192 + 128], rhs=cq[:, kc, c0:c0 + 512], start=(kc == 0), stop=(kc == 2)),
                             reads=["wuq", "cq"], writes=[f"ps{bi}"])
                    P.op("scalar", lambda e, c0=c0, pb=pb: e.mul(out=QN[:, c0:c0 + 512], in_=pb[:, :], mul=SC), reads=[f"ps{bi}"], writes=["QN"])
                    bi2 = 5 + (ci + 1) % 3
                    pb2 = bank(K, bi2)
                    for kc in range(3):
                        P.op("tensor", lambda e, kc=kc, c0=c0, pb2=pb2, h=h: e.matmul(pb2[:, :], lhsT=wpe[:, kc, h, :], rhs=cq[:, kc, c0:c0 + 512], start=(kc == 0), stop=(kc == 2)),
                             reads=["wpe", "cq"], writes=[f"ps{bi2}"])
                    P.op("scalar", lambda e, pb2=pb2: e.mul(out=xq[:], in_=pb2[:, :], mul=SC), reads=[f"ps{bi2}"], writes=["xq"])
                    bi3 = 5 + (ci + 2) % 3
                    pb3 = bank(K, bi3)
                    P.op("tensor", lambda e, pb3=pb3: e.matmul(pb3[:, :], lhsT=rRT[:, :], rhs=xq[:, :], start=True, stop=True), reads=["xq", "rRT"], writes=[f"ps{bi3}"])
                    P.op("vector", lambda e, c0=c0: e.tensor_tensor(out=t1[:], in0=xq[:], in1=rcos[:, c0:c0 + 512], op=ALU.mult), reads=["xq", "rcos"], writes=["t1"])
                    P.op("vector", lambda e, c0=c0, pb3=pb3: e.tensor_tensor(out=t2[:], in0=pb3[:, :], in1=rsin[:, c0:c0 + 512], op=ALU.mult), reads=[f"ps{bi3}", "rsin"], writes=["t2"])
                    P.op("vector", lambda e, c0=c0: e.tensor_tensor(out=QP[:, c0:c0 + 512], in0=t1[:], in1=t2[:], op=ALU.add), reads=["t1", "t2"], writes=["QP"])
                def emit_S(qt):
                    qs = slice(qt * 128, (qt + 1) * 128)
                    for ci, c0 in enumerate(range(0, LT, 512)):
                        n = min(512, LT - c0)
                        pb = bank(K, ci)
                        P.op("tensor", lambda e, c0=c0, n=n, pb=pb, qs=qs: e.matmul(pb[:, 0:n], lhsT=QN[:, qs], rhs=KT[:, c0:c0 + n], start=True, stop=False), reads=["QN", "KT"], writes=[f"ps{ci}"])
                    for ci, c0 in enumerate(range(0, LT, 512)):
                        n = min(512, LT - c0)
                        pb = bank(K, ci)
                        P.op("tensor", lambda e, c0=c0, n=n, pb=pb, qs=qs: e.matmul(pb[:, 0:n], lhsT=QP[:, qs], rhs=kpe[:, c0:c0 + n], start=False, stop=True), reads=["QP", "kpe"], writes=[f"ps{ci}"])

                S = K.psum[:, 0:LT]

                def softmax_a(qt):
                    o = (qt % 4) * 4
                    pmb = Pm[qt % 2]
                    P.op("vector", lambda e, o=o: e.tensor_reduce(out=sm[:, o:o + 1], in_=S, axis=AX.X, op=ALU.max, negate=True), reads=SK, writes=[f"sm{o}"])
                    P.op("scalar", lambda e, o=o, pmb=pmb: e.activation(out=pmb[:, :], in_=S, func=AF.Exp, bias=sm[:, o:o + 1], scale=1.0, accum_out=sm[:, o + 1:o + 2]), reads=SK + [f"sm{o}"], writes=[f"Pm{qt % 2}", f"sm{o + 1}"])

                def softmax_b(qt):
                    o = (qt % 4) * 4
                    P.op("vector", lambda e, o=o: e.reciprocal(out=sm[:, o + 2:o + 3], in_=sm[:, o + 1:o + 2]), reads=[f"sm{o + 1}"], writes=[f"sm{o + 2}"])

                def emit_T(qt):
                    pmb = Pm[qt % 2]
                    ptb = PT[qt % 2]
                    for g4 in range(0, NCH, 4):
                        ng = min(4, NCH - g4)
                        bi = 5 + (g4 // 4) % 2
                        pb = bank(K, bi)
                        for tt in range(ng):
                            P.op("tensor", lambda e, tt=tt, g4=g4, pb=pb, pmb=pmb: e.transpose(out=pb[:, tt * 128:(tt + 1) * 128], in_=pmb[:, (g4 + tt) * 128:(g4 + tt + 1) * 128], identity=K.ident[:]),
                                 reads=[f"Pm{qt % 2}", "ident"], writes=[f"ps{bi}"])
                        evac(K, ptb[:, g4:g4 + ng, :], pb[:, 0:ng * 128], [f"ps{bi}"], [f"PT{qt % 2}"])

                def emit_PV(qt):
                    o = (qt % 4) * 4
                    ptb = PT[qt % 2]
                    pb = bank(K, 7)
                    for kt in range(NCH):
                        P.op("tensor", lambda e, kt=kt, pb=pb, ptb=ptb: e.matmul(pb[:, 0:128], lhsT=ptb[:, kt, :], rhs=V[:, kt, :], start=(kt == 0), stop=(kt == NCH - 1)), reads=[f"PT{qt % 2}", "V"], writes=["ps7"])
                    P.op("scalar", lambda e, qt=qt, pb=pb, o=o: e.activation(out=atth[:, qt, :], in_=pb[:, 0:128], func=AF.Identity, scale=sm[:, o + 2:o + 3]), reads=["ps7", f"sm{o + 2}"], writes=["atth"])

                emit_S(0)
                softmax_a(0)
                softmax_b(0)
                for qt in range(16):
                    if qt + 1 < 16:
                        emit_S(qt + 1)
                        softmax_a(qt + 1)
                    if qt >= 1:
                        emit_PV(qt - 1)
                    emit_T(qt)
                    if qt + 1 < 16:
                        softmax_b(qt + 1)
                emit_PV(15)
                dst = A["att"][b].rearrange("(qt p) (hh d) -> p qt hh d", p=128, d=128)[:, :, h, :]
                P.dma("gpsimd", dst, atth[:], reads=["atth"], writes=["d_att"])
        end_phase(K)


def reduce_turns(K, w, n, tv, ti, tf, tm, key, shift=0.0):
    P = K.P
    P.op("vector", lambda e: e.tensor_scalar(out=tv[:, 0:n], in0=w, scalar1=float(shift), scalar2=None, op0=ALU.add), reads=[key], writes=["tv"])
    P.op("vector", lambda e: e.tensor_copy(out=ti[:, 0:n], in_=tv[:, 0:n]), reads=["tv"], writes=["ti"])
    P.op("vector", lambda e: e.tensor_copy(out=tf[:, 0:n], in_=ti[:, 0:n]), reads=["ti"], writes=["tf"])
    P.op("vector", lambda e: e.tensor_tensor(out=tv[:, 0:n], in0=tv[:, 0:n], in1=tf[:, 0:n], op=ALU.subtract), reads=["tv", "tf"], writes=["tv"])
    P.op("vector", lambda e: e.tensor_scalar(out=tm[:, 0:n], in0=tv[:, 0:n], scalar1=0.5, scalar2=None, op0=ALU.is_gt), reads=["tv"], writes=["tm"])
    P.op("vector", lambda e: e.tensor_tensor(out=tv[:, 0:n], in0=tv[:, 0:n], in1=tm[:, 0:n], op=ALU.subtract), reads=["tv", "tm"], writes=["tv"])
    P.op("vector", lambda e: e.tensor_scalar(out=tm[:, 0:n], in0=tv[:, 0:n], scalar1=-0.5, scalar2=None, op0=ALU.is_lt), reads=["tv"], writes=["tm"])
    P.op("vector", lambda e: e.tensor_tensor(out=tv[:, 0:n], in0=tv[:, 0:n], in1=tm[:, 0:n], op=ALU.add), reads=["tv", "tm"], writes=["tv"])


def sincos_turns(K, w, n, out_cos, out_sin, tmps, key, okeys):
    tv, ti, tf, tm = tmps
    P = K.P
    reduce_turns(K, w, n, tv, ti, tf, tm, key, 0.0)
    P.op("scalar", lambda e: e.activation(out=out_sin, in_=tv[:, 0:n], func=AF.Sin, scale=TWO_PI), reads=["tv"], writes=[okeys[1]])
    reduce_turns(K, w, n, tv, ti, tf, tm, key, 0.25)
    P.op("scalar", lambda e: e.activation(out=out_cos, in_=tv[:, 0:n], func=AF.Sin, scale=TWO_PI), reads=["tv"], writes=[okeys[0]])


def accurate_exp(K, x, n, out, tmps, xkey, okey):
    P = K.P
    tv, ti, tf, tm = tmps
    LN2_HI, LN2_LO = 0.693145751953125, 1.428606765330187e-06
    V = lambda f_, reads, writes: P.op("vector", f_, reads=reads, writes=writes)
    V(lambda e: e.tensor_scalar(out=tv[:, 0:n], in0=x, scalar1=1.0 / math.log(2.0), scalar2=None, op0=ALU.mult), [xkey], ["tv"])
    V(lambda e: e.tensor_copy(out=ti[:, 0:n], in_=tv[:, 0:n]), ["tv"], ["ti"])
    V(lambda e: e.tensor_copy(out=tf[:, 0:n], in_=ti[:, 0:n]), ["ti"], ["tf"])
    V(lambda e: e.scalar_tensor_tensor(out=tv[:, 0:n], in0=tf[:, 0:n], scalar=-LN2_HI, in1=x, op0=ALU.mult, op1=ALU.add), ["tf", xkey], ["tv"])
    V(lambda e: e.scalar_tensor_tensor(out=tv[:, 0:n], in0=tf[:, 0:n], scalar=-LN2_LO, in1=tv[:, 0:n], op0=ALU.mult, op1=ALU.add), ["tf", "tv"], ["tv"])
    V(lambda e: e.memset(tm[:, 0:n], 1.0), [], ["tm"])
    for d_ in range(12, 0, -1):
        V(lambda e: e.tensor_tensor(out=tm[:, 0:n], in0=tm[:, 0:n], in1=tv[:, 0:n], op=ALU.mult), ["tm", "tv"], ["tm"])
        V(lambda e, d_=d_: e.tensor_scalar(out=tm[:, 0:n], in0=tm[:, 0:n], scalar1=1.0 / d_, scalar2=1.0, op0=ALU.mult, op1=ALU.add), ["tm"], ["tm"])
    V(lambda e: e.tensor_scalar(out=tf[:, 0:n], in0=tf[:, 0:n], scalar1=127.0, scalar2=None, op0=ALU.add), ["tf"], ["tf"])
    V(lambda e: e.tensor_copy(out=ti[:, 0:n], in_=tf[:, 0:n]), ["tf"], ["ti"])
    V(lambda e: e.tensor_single_scalar(out=ti[:, 0:n], in_=ti[:, 0:n], scalar=23, op=ALU.logical_shift_left), ["ti"], ["ti"])
    V(lambda e: e.tensor_tensor(out=out, in0=tm[:, 0:n], in1=ti[:, 0:n].bitcast(F32), op=ALU.mult), ["tm", "ti"], [okey])


def phase3(K):
    nc, P, A = K.nc, K.P, K.A
    INV2PI = 1.0 / (2.0 * math.pi)
    with ExitStack() as ph:
        sb = lambda n, s, dt=F32: ph.enter_context(nc.sbuf_tensor(f"q7_{n}", s, dt))
        cs = sb("cs", [128, 32, T]); sn = sb("sn", [128, 32, T])
        E3c = sb("E3c", [128, 32, NCH]); E3s = sb("E3s", [128, 32, NCH]); E4c = sb("E4c", [128, 32, NCH]); E4s = sb("E4s", [128, 32, NCH])
        coefT = sb("coefT", [128, 32, NCH])
        rr_ = sb("r", [128, 32])
        BW = sb("BW", [128, 2, 2, 4, 128])
        cre1 = sb("cre1", [128, 2, 16, 16]); cimn = sb("cimn", [128, 2, 16, 16])
        stg = [[sb(f"stg{p_}_{ri}", [128, 128]) for ri in range(2)] for p_ in range(4)]
        cmask = sb("cmask", [128, LT]); pmask = sb("pmask", [128, 4]); sd = sb("sd", [128, 4])
        P.dma("sync", cmask[:], A["cmask"], writes=["cmask"]); P.dma("sync", pmask[:], A["pmask"], writes=["pmask"]); P.dma("sync", sd[:], A["s_d"], writes=["sd"])
        P.dma("sync", cre1[:], A["s_cre1"], writes=["cre1"]); P.dma("scalar", cimn[:], A["s_cim1"], writes=["cimn"])
        P.op("vector", lambda e: e.tensor_scalar(out=cimn[:], in0=cimn[:], scalar1=-1.0, scalar2=None, op0=ALU.mult), reads=["cimn"], writes=["cimn"])
        for p_ in range(4):
            for ri in range(2):
                P.op("gpsimd", lambda e, p_=p_, ri=ri: e.memset(stg[p_][ri][:], 0.0), writes=[f"stg{p_}_{ri}"])
        with ExitStack() as ts:
            tb = lambda n, s, dt=F32: ts.enter_context(nc.sbuf_tensor(f"q3t_{n}", s, dt))
            tv = tb("tv", [128, 4096]); ti = tb("ti", [128, 4096], I32); tf = tb("tf", [128, 4096]); tm = tb("tm", [128, 4096])
            tmps = (tv, ti, tf, tm)
            wbig = tb("wbig", [128, 4096])
            iota_s = tb("iota_s", [128, T]); iota_c = tb("iota_c", [128, NCH])
            P.dma("sync", iota_s[:], A["iota_s"], writes=["iota_s"]); P.dma("sync", iota_c[:], A["iota_c"], writes=["iota_c"])
            a1 = tb("a1", [128, 32]); i1 = tb("i1", [128, 32]); l1 = tb("l1", [128, 32])
            P.dma("sync", a1[:], A["s_are1"].rearrange("p d r -> p (d r)"), writes=["a1"])
            P.dma("scalar", i1[:], A["s_aim1"].rearrange("p d r -> p (d r)"), writes=["i1"])
            P.dma("sync", l1[:], A["s_ldt1"].rearrange("p d r -> p (d r)"), writes=["l1"])
            dt1 = tb("dt1", [128, 32]); ardt = tb("ardt", [128, 32]); w1 = tb("w1", [128, 32]); phiT = tb("phiT", [128, 32]); base3 = tb("base3", [128, 32])
            wc = tb("wc", [128, 32, NCH]); w34 = tb("w34", [128, 32, NCH])
            accurate_exp(K, l1[:], 32, dt1[:], tmps, "l1", "dt1")
            P.op("vector", lambda e: e.tensor_tensor(out=ardt[:], in0=a1[:], in1=dt1[:], op=ALU.mult), reads=["a1", "dt1"], writes=["ardt"])
            P.op("vector", lambda e: e.tensor_tensor(out=w1[:], in0=i1[:], in1=dt1[:], op=ALU.mult), reads=["i1", "dt1"], writes=["w1"])
            P.op("vector", lambda e: e.tensor_scalar(out=w1[:], in0=w1[:], scalar1=INV2PI, scalar2=None, op0=ALU.mult), reads=["w1"], writes=["w1"])
            P.op("scalar", lambda e: e.activation(out=rr_[:], in_=ardt[:], func=AF.Exp), reads=["ardt"], writes=["r"])
            P.op("scalar", lambda e: e.activation(out=l1[:], in_=ardt[:], func=AF.Exp, scale=float(T)), reads=["ardt"], writes=["l1"])
            P.op("vector", lambda e: e.tensor_copy(out=coefT[:], in_=l1[:].unsqueeze(2).to_broadcast([128, 32, NCH])), reads=["l1"], writes=["coefT"])
            P.op("vector", lambda e: e.tensor_tensor(out=wbig[:].rearrange("p (a s) -> p a s", s=T), in0=w1[:].unsqueeze(2).to_broadcast([128, 32, T]),
                                                     in1=iota_s[:].unsqueeze(1).to_broadcast([128, 32, T]), op=ALU.mult), reads=["w1", "iota_s"], writes=["wbig"])
            sincos_turns(K, wbig[:], 4096, cs[:].rearrange("p a s -> p (a s)"), sn[:].rearrange("p a s -> p (a s)"), tmps, "wbig", ("cs", "sn"))
            P.op("vector", lambda e: e.tensor_scalar(out=phiT[:], in0=w1[:], scalar1=float(T), scalar2=None, op0=ALU.mult), reads=["w1"], writes=["phiT"])
            reduce_turns(K, phiT[:], 32, tv, ti, tf, tm, "phiT", 0.0)
            P.op("vector", lambda e: e.tensor_copy(out=phiT[:], in_=tv[:, 0:32]), reads=["tv"], writes=["phiT"])
            P.op("vector", lambda e: e.tensor_tensor(out=base3[:], in0=phiT[:], in1=w1[:], op=ALU.subtract), reads=["phiT", "w1"], writes=["base3"])
            P.op("vector", lambda e: e.tensor_tensor(out=wc[:], in0=phiT[:].unsqueeze(2).to_broadcast([128, 32, NCH]), in1=iota_c[:].unsqueeze(1).to_broadcast([128, 32, NCH]), op=ALU.mult),
                 reads=["phiT", "iota_c"], writes=["wc"])
            P.op("vector", lambda e: e.tensor_tensor(out=w34[:], in0=base3[:].unsqueeze(2).to_broadcast([128, 32, NCH]), in1=wc[:], op=ALU.subtract), reads=["base3", "wc"], writes=["w34"])
            sincos_turns(K, w34[:].rearrange("p a c -> p (a c)"), 32 * NCH, E3c[:].rearrange("p a c -> p (a c)"), E3s[:].rearrange("p a c -> p (a c)"), tmps, "w34", ("E3c", "E3s"))
            P.op("vector", lambda e: e.tensor_tensor(out=w34[:], in0=w1[:].unsqueeze(2).to_broadcast([128, 32, NCH]), in1=wc[:], op=ALU.add), reads=["w1", "wc"], writes=["w34"])
            sincos_turns(K, w34[:].rearrange("p a c -> p (a c)"), 32 * NCH, E4c[:].rearrange("p a c -> p (a c)"), E4s[:].rearrange("p a c -> p (a c)"), tmps, "w34", ("E4c", "E4s"))
            for (tb_, k_) in ((E4c, "E4c"), (E4s, "E4s")):
                P.op("vector", lambda e, tb_=tb_: e.tensor_tensor(out=tb_[:], in0=tb_[:], in1=rr_[:].unsqueeze(2).to_broadcast([128, 32, NCH]), op=ALU.mult), reads=[k_, "r"], writes=[k_])
            a2 = tb("a2", [128, 512]); i2 = tb("i2", [128, 512]); l2 = tb("l2", [128, 512]); br2 = tb("br2", [128, 512]); bi2 = tb("bi2", [128, 512])
            for (t_, nm, q_) in ((a2, "s_are2", "sync"), (i2, "s_aim2", "scalar"), (l2, "s_ldt2", "sync"), (br2, "s_bre2", "scalar"), (bi2, "s_bim2", "sync")):
                P.dma(q_, t_[:], A[nm].rearrange("p d j q -> p (d j q)"), writes=[nm])
            x1 = tb("x1", [128, 512]); x2 = tb("x2", [128, 512]); x3 = tb("x3", [128, 512]); x4 = tb("x4", [128, 512]); x5 = tb("x5", [128, 512]); x6 = tb("x6", [128, 512])
            V_ = lambda f_, **kw: P.op("vector", f_, **kw)
            accurate_exp(K, l2[:], 512, l2[:], tmps, "s_ldt2", "s_ldt2")
            V_(lambda e: e.tensor_tensor(out=x1[:], in0=a2[:], in1=l2[:], op=ALU.mult), reads=["s_are2", "s_ldt2"], writes=["x1"])
            V_(lambda e: e.tensor_tensor(out=x2[:], in0=i2[:], in1=l2[:], op=ALU.mult), reads=["s_aim2", "s_ldt2"], writes=["x2"])
            V_(lambda e: e.tensor_scalar(out=x2[:], in0=x2[:], scalar1=INV2PI, scalar2=None, op0=ALU.mult), reads=["x2"], writes=["x2"])
            P.op("scalar", lambda e: e.activation(out=x1[:], in_=x1[:], func=AF.Exp), reads=["x1"], writes=["x1"])
            sincos_turns(K, x2[:], 512, x3[:], x4[:], tmps, "x2", ("x3", "x4"))
            V_(lambda e: e.tensor_tensor(out=x3[:], in0=x3[:], in1=x1[:], op=ALU.mult), reads=["x3", "x1"], writes=["x3"])
            V_(lambda e: e.tensor_tensor(out=x4[:], in0=x4[:], in1=x1[:], op=ALU.mult), reads=["x4", "x1"], writes=["x4"])
            V_(lambda e: e.tensor_scalar(out=x3[:], in0=x3[:], scalar1=-1.0, scalar2=None, op0=ALU.add), reads=["x3"], writes=["x3"])
            V_(lambda e: e.tensor_tensor(out=x1[:], in0=a2[:], in1=a2[:], op=ALU.mult), reads=["s_are2"], writes=["x1"])
            V_(lambda e: e.tensor_tensor(out=x2[:], in0=i2[:], in1=i2[:], op=ALU.mult), reads=["s_aim2"], writes=["x2"])
            V_(lambda e: e.tensor_tensor(out=x1[:], in0=x1[:], in1=x2[:], op=ALU.add), reads=["x1", "x2"], writes=["x1"])
            V_(lambda e: e.reciprocal(out=x1[:], in_=x1[:]), reads=["x1"], writes=["x1"])
            V_(lambda e: e.tensor_tensor(out=x5[:], in0=x3[:], in1=a2[:], op=ALU.mult), reads=["x3", "s_are2"], writes=["x5"])
            V_(lambda e: e.tensor_tensor(out=x6[:], in0=x4[:], in1=i2[:], op=ALU.mult), reads=["x4", "s_aim2"], writes=["x6"])
            V_(lambda e: e.tensor_tensor(out=x5[:], in0=x5[:], in1=x6[:], op=ALU.add), reads=["x5", "x6"], writes=["x5"])
            V_(lambda e: e.tensor_tensor(out=x5[:], in0=x5[:], in1=x1[:], op=ALU.mult), reads=["x5", "x1"], writes=["x5"])
            V_(lambda e: e.tensor_tensor(out=x6[:], in0=x4[:], in1=a2[:], op=ALU.mult), reads=["x4", "s_are2"], writes=["x6"])
            V_(lambda e: e.tensor_tensor(out=x2[:], in0=x3[:], in1=i2[:], op=ALU.mult), reads=["x3", "s_aim2"], writes=["x2"])
            V_(lambda e: e.tensor_tensor(out=x6[:], in0=x6[:], in1=x2[:], op=ALU.subtract), reads=["x6", "x2"], writes=["x6"])
            V_(lambda e: e.tensor_tensor(out=x6[:], in0=x6[:], in1=x1[:], op=ALU.mult), reads=["x6", "x1"], writes=["x6"])
            V_(lambda e: e.tensor_tensor(out=x3[:], in0=x5[:], in1=br2[:], op=ALU.mult), reads=["x5", "s_bre2"], writes=["x3"])
            V_(lambda e: e.tensor_tensor(out=x2[:], in0=x6[:], in1=bi2[:], op=ALU.mult), reads=["x6", "s_bim2"], writes=["x2"])
            V_(lambda e: e.tensor_tensor(out=x3[:], in0=x3[:], in1=x2[:], op=ALU.subtract), reads=["x3", "x2"], writes=["x3"])
            V_(lambda e: e.tensor_tensor(out=x4[:], in0=x5[:], in1=bi2[:], op=ALU.mult), reads=["x5", "s_bim2"], writes=["x4"])
            V_(lambda e: e.tensor_tensor(out=x2[:], in0=x6[:], in1=br2[:], op=ALU.mult), reads=["x6", "s_bre2"], writes=["x2"])
            V_(lambda e: e.tensor_tensor(out=x4[:], in0=x4[:], in1=x2[:], op=ALU.add), reads=["x4", "x2"], writes=["x4"])
            for ri, (src, sk) in enumerate(((x3, "x3"), (x4, "x4"))):
                for g2 in range(2):
                    V_(lambda e, ri=ri, g2=g2, src=src: e.tensor_scalar(out=BW[:, :, ri, :, g2 * 64:(g2 + 1) * 64], in0=src[:].rearrange("p (d j q) -> p d j q", d=2, j=4),
                                                                       scalar1=pmask[:, g2:g2 + 1], scalar2=None, op0=ALU.mult), reads=[sk, "pmask"], writes=["BW"])
            P.barrier()
            P.flush()
        coef = [sb(f"coef{i}", [128, LT]) for i in range(2)]
        uf = sb("uf", [128, LT]); ur = sb("ur", [128, LT])
        btr = sb("btr", [128, LT]); bti = sb("bti", [128, LT])
        gr = [sb(f"gr{i}", [128, LT]) for i in range(2)]
        gi = [sb(f"gi{i}", [128, LT]) for i in range(2)]
        hr = sb("hr", [128, L]); hi = sb("hi", [128, L])
        t1 = sb("t1", [128, 512]); t2 = sb("t2", [128, 512]); t3 = sb("t3", [128, L]); t4 = sb("t4", [128, L])
        ytot = sb("ytot", [128, L])
        bx = sb("bx", [128, 8, NCH])
        YK = ["ps4", "ps5", "ps6", "ps7"]
        itb = [0]
        items = [(b, j, d, pos) for b in range(K.nb) for j in range(4) for d in range(2) for pos in range(4)]

        def stage_A(n):
            b, j, d, pos = items[n]
            p2 = n % 2
            if d == 0 and pos == 0:
                P.dma("sync", uf[:], A["uT"][b, j * 128:(j + 1) * 128, :], reads=["d_uT"], writes=["uf"])
                P.op("gpsimd", lambda e: e.tensor_copy(out=ur[:, 0:LC], in_=uf[:, LC - 1::-1]), reads=["uf"], writes=["ur"])
                P.op("gpsimd", lambda e: e.tensor_copy(out=ur[:, LC:LT], in_=uf[:, LT - 1:LC - 1:-1]), reads=["uf"], writes=["ur"])
            u = uf if d == 0 else ur
            uk = "uf" if d == 0 else "ur"
            rb = j * 4 + pos
            a_ = d * 16 + rb
            for ri, csrc in enumerate((cre1, cimn)):
                for g2 in range(2):
                    P.op("gpsimd", lambda e, ri=ri, g2=g2, csrc=csrc, pos=pos, d=d, rb=rb: e.tensor_scalar(
                        out=stg[pos][ri][:, pos * 32 + g2 * 16:pos * 32 + (g2 + 1) * 16], in0=csrc[:, d, rb, :], scalar1=pmask[:, 2 + g2:3 + g2], scalar2=None, op0=ALU.mult),
                        reads=["cre1", "cimn", "pmask"], writes=[f"stg{pos}_{ri}"])
            P.op("scalar", lambda e, a_=a_, p2=p2: e.activation(out=coef[p2][:], in_=cmask[:], func=AF.Identity, scale=rr_[:, a_:a_ + 1]), reads=["cmask", "r"], writes=[f"coef{p2}"])
            for ci, c0 in enumerate(range(0, LT, 512)):
                n_ = min(512, LT - c0)
                nk = n_ // 128
                b0_ = (itb[0] % 2) * 2
                itb[0] += 1
                pre, pim = bank(K, b0_), bank(K, b0_ + 1)
                kre, kim = f"ps{b0_}", f"ps{b0_ + 1}"
                rows = slice(pos * 32, (pos + 1) * 32)
                P.op("tensor", lambda e, pre=pre, n_=n_, rows=rows, d=d, j=j, u=u, c0=c0, pos=pos: e.matmul(pre[:, 0:n_], lhsT=BW[rows, d, 0, j, :], rhs=u[rows, c0:c0 + n_], start=True, stop=True, tile_position=(pos * 32, 0)),
                     reads=["BW", uk], writes=[kre])
                P.op("tensor", lambda e, pim=pim, n_=n_, rows=rows, d=d, j=j, u=u, c0=c0, pos=pos: e.matmul(pim[:, 0:n_], lhsT=BW[rows, d, 1, j, :], rhs=u[rows, c0:c0 + n_], start=True, stop=True, tile_position=(pos * 32, 0)),
                     reads=["BW", uk], writes=[kim])
                v3 = lambda ap_: ap_.rearrange("p (c s) -> p c s", s=T)
                csb = cs[:, a_:a_ + 1, :].to_broadcast([128, nk, T])
                snb = sn[:, a_:a_ + 1, :].to_broadcast([128, nk, T])
                obr, obi = btr[:, c0:c0 + n_], bti[:, c0:c0 + n_]
                P.op("vector", lambda e, csb=csb, pre=pre, n_=n_, v3=v3, obr=obr: e.tensor_tensor(out=v3(obr), in0=csb, in1=v3(pre[:, 0:n_]), op=ALU.mult), reads=["cs", kre], writes=["btr"])
                P.op("vector", lambda e, snb=snb, pim=pim, n_=n_, v3=v3: e.tensor_tensor(out=v3(t1[:, 0:n_]), in0=snb, in1=v3(pim[:, 0:n_]), op=ALU.mult), reads=["sn", kim], writes=["t1"])
                P.op("vector", lambda e, csb=csb, pim=pim, n_=n_, v3=v3, obi=obi: e.tensor_tensor(out=v3(obi), in0=csb, in1=v3(pim[:, 0:n_]), op=ALU.mult), reads=["cs", kim], writes=["bti"])
                P.op("vector", lambda e, snb=snb, pre=pre, n_=n_, v3=v3: e.tensor_tensor(out=v3(t2[:, 0:n_]), in0=snb, in1=v3(pre[:, 0:n_]), op=ALU.mult), reads=["sn", kre], writes=["t2"])
                P.op("vector", lambda e, n_=n_, obr=obr: e.tensor_tensor(out=obr, in0=obr, in1=t1[:, 0:n_], op=ALU.add), reads=["btr", "t1"], writes=["btr"])
                P.op("vector", lambda e, n_=n_, obi=obi: e.tensor_tensor(out=obi, in0=obi, in1=t2[:, 0:n_], op=ALU.subtract), reads=["bti", "t2"], writes=["bti"])

        def stage_B(n):
            b, j, d, pos = items[n]
            p2 = n % 2
            rb = j * 4 + pos
            a_ = d * 16 + rb
            g_r, g_i, cf = gr[p2], gi[p2], coef[p2]
            kgr, kgi, kcf = f"gr{p2}", f"gi{p2}", f"coef{p2}"
            P.op("vector", lambda e: e.tensor_tensor_scan(out=g_r[:], data0=cf[:], data1=btr[:], initial=0.0, op0=ALU.mult, op1=ALU.add), reads=[kcf, "btr"], writes=[kgr])
            P.op("vector", lambda e: e.tensor_tensor_scan(out=g_i[:], data0=cf[:], data1=bti[:], initial=0.0, op0=ALU.mult, op1=ALU.add), reads=[kcf, "bti"], writes=[kgi])
            ger, gei = g_r[:, T - 1:LT:T], g_i[:, T - 1:LT:T]
            e3c, e3s, e4c, e4s, cT = E3c[:, a_, :], E3s[:, a_, :], E4c[:, a_, :], E4s[:, a_, :], coefT[:, a_, :]
            TT = lambda out, in0, in1, op, reads, writes: P.op("vector", lambda e: e.tensor_tensor(out=out, in0=in0, in1=in1, op=op), reads=reads, writes=writes)
            TT(bx[:, 0, :], e3c, ger, ALU.mult, ["E3c", kgr], ["bx0"])
            TT(bx[:, 1, :], e3s, gei, ALU.mult, ["E3s", kgi], ["bx1"])
            TT(bx[:, 2, :], e3c, gei, ALU.mult, ["E3c", kgi], ["bx2"])
            TT(bx[:, 3, :], e3s, ger, ALU.mult, ["E3s", kgr], ["bx3"])
            TT(bx[:, 0, :], bx[:, 0, :], bx[:, 1, :], ALU.subtract, ["bx0", "bx1"], ["bx0"])
            TT(bx[:, 2, :], bx[:, 2, :], bx[:, 3, :], ALU.add, ["bx2", "bx3"], ["bx2"])
            P.op("vector", lambda e: e.tensor_tensor_scan(out=bx[:, 4, :], data0=cT, data1=bx[:, 0, :], initial=0.0, op0=ALU.mult, op1=ALU.add), reads=["coefT", "bx0"], writes=["bx4"])
            P.op("vector", lambda e: e.tensor_tensor_scan(out=bx[:, 5, :], data0=cT, data1=bx[:, 2, :], initial=0.0, op0=ALU.mult, op1=ALU.add), reads=["coefT", "bx2"], writes=["bx5"])
            TT(bx[:, 0, :], e4c, bx[:, 4, :], ALU.mult, ["E4c", "bx4"], ["bx0"])
            TT(bx[:, 1, :], e4s, bx[:, 5, :], ALU.mult, ["E4s", "bx5"], ["bx1"])
            TT(bx[:, 2, :], e4c, bx[:, 5, :], ALU.mult, ["E4c", "bx5"], ["bx2"])
            TT(bx[:, 3, :], e4s, bx[:, 4, :], ALU.mult, ["E4s", "bx4"], ["bx3"])
            TT(bx[:, 6, :], bx[:, 0, :], bx[:, 1, :], ALU.subtract, ["bx0", "bx1"], ["bx6"])
            TT(bx[:, 7, :], bx[:, 2, :], bx[:, 3, :], ALU.add, ["bx2", "bx3"], ["bx7"])
            TT(btr[:, T:LT:T], btr[:, T:LT:T], bx[:, 6, 0:NCH - 1], ALU.add, ["btr", "bx6"], ["btr"])
            TT(bti[:, T:LT:T], bti[:, T:LT:T], bx[:, 7, 0:NCH - 1], ALU.add, ["bti", "bx7"], ["bti"])
            P.op("vector", lambda e: e.tensor_tensor_scan(out=g_r[:, LC:LT], data0=cf[:, LC:LT], data1=btr[:, LC:LT], initial=0.0, op0=ALU.mult, op1=ALU.add), reads=[kcf, "btr"], writes=[kgr])
            P.op("vector", lambda e: e.tensor_tensor_scan(out=g_i[:, LC:LT], data0=cf[:, LC:LT], data1=bti[:, LC:LT], initial=0.0, op0=ALU.mult, op1=ALU.add), reads=[kcf, "bti"], writes=[kgi])

        def stage_C(n):
            b, j, d, pos = items[n]
            p2 = n % 2
            rb = j * 4 + pos
            a_ = d * 16 + rb
            g_r, g_i = gr[p2], gi[p2]
            kgr, kgi = f"gr{p2}", f"gi{p2}"
            v3l = lambda ap_: ap_.rearrange("p (c s) -> p c s", s=T)
            csl = cs[:, a_:a_ + 1, :].to_broadcast([128, 16, T])
            snl = sn[:, a_:a_ + 1, :].to_broadcast([128, 16, T])
            G_ = lambda out, in0, in1, op, reads, writes: P.op("vector", lambda e: e.tensor_tensor(out=out, in0=in0, in1=in1, op=op), reads=reads, writes=writes)
            G_(v3l(hr[:]), v3l(g_r[:, LC:LT]), csl, ALU.mult, ["cs", kgr], ["hr"])
            P.op("vector", lambda e: e.scalar_tensor_tensor(out=v3l(t3[:]), in0=v3l(g_i[:, LC:LT]), scalar=-1.0, in1=snl, op0=ALU.mult, op1=ALU.mult), reads=["sn", kgi], writes=["t3"])
            G_(v3l(hi[:]), v3l(g_i[:, LC:LT]), csl, ALU.mult, ["cs", kgi], ["hi"])
            G_(v3l(t4[:]), v3l(g_r[:, LC:LT]), snl, ALU.mult, ["sn", kgr], ["t4"])

        def stage_D(n):
            b, j, d, pos = items[n]
            for nt in range(4):
                yb = bank(K, 4 + nt)
                P.op("tensor", lambda e, yb=yb, nt=nt, pos=pos: e.matmul(yb[:, :], lhsT=stg[pos][0][:, :], rhs=hr[:, nt * 512:(nt + 1) * 512], start=(pos == 0), stop=False),
                     reads=[f"stg{pos}_0", "hr"], writes=[YK[nt]])
                P.op("tensor", lambda e, yb=yb, nt=nt, pos=pos: e.matmul(yb[:, :], lhsT=stg[pos][0][:, :], rhs=t3[:, nt * 512:(nt + 1) * 512], start=False, stop=False),
                     reads=[f"stg{pos}_0", "t3"], writes=[YK[nt]])
                P.op("tensor", lambda e, yb=yb, nt=nt, pos=pos: e.matmul(yb[:, :], lhsT=stg[pos][1][:, :], rhs=hi[:, nt * 512:(nt + 1) * 512], start=False, stop=False),
                     reads=[f"stg{pos}_1", "hi"], writes=[YK[nt]])
                P.op("tensor", lambda e, yb=yb, nt=nt, pos=pos: e.matmul(yb[:, :], lhsT=stg[pos][1][:, :], rhs=t4[:, nt * 512:(nt + 1) * 512], start=False, stop=(pos == 3)),
                     reads=[f"stg{pos}_1", "t4"], writes=[YK[nt]])
            if pos == 3:
                Y = K.psum[:, 2048:4096]
                if d == 0:
                    P.op("vector", lambda e, j=j, Y=Y: e.scalar_tensor_tensor(out=ytot[:], in0=uf[:, LC:LT], scalar=sd[:, j:j + 1], in1=Y, op0=ALU.mult, op1=ALU.add), reads=["uf", "sd"] + YK, writes=["ytot"])
                else:
                    P.op("vector", lambda e, Y=Y: e.tensor_tensor(out=ytot[:], in0=ytot[:], in1=Y[:, ::-1], op=ALU.add), reads=["ytot"] + YK, writes=["ytot"])
                    P.op("scalar", lambda e: e.activation(out=ytot[:], in_=ytot[:], func=AF.Gelu_apprx_tanh), reads=["ytot"], writes=["ytot"])
                    P.dma("scalar", A["gyT"][b, j * 128:(j + 1) * 128, :], ytot[:], reads=["ytot"], writes=["d_gyT"])

        NI = len(items)
        stage_A(0)
        for n in range(NI):
            stage_B(n)
            stage_C(n)
            if n + 1 < NI:
                nb_, nj_, nd_, npos_ = items[n + 1]
                if nd_ == 0 and npos_ == 0:
                    stage_D(n)
                    stage_A(n + 1)
                else:
                    stage_A(n + 1)
                    stage_D(n)
            else:
                stage_D(n)
        end_phase(K)


def phase4(K):
    nc, P, A = K.nc, K.P, K.A
    with ExitStack() as ph:
        sb = lambda n, s, dt=F32: ph.enter_context(nc.sbuf_tensor(f"q4_{n}", s, dt))
        wglu = sb("wglu", [128, 4, 2048]); wout = sb("wout", [128, 8, D])
        P.dma("sync", wglu[:], A["w_glu"].rearrange("(kc p) n -> p kc n", p=128), writes=["wglu"])
        P.dma("scalar", wout[:], A["w_out"].rearrange("(kc p) n -> p kc n", p=128), writes=["wout"])
        lg = sb("lg", [128, D]); lb = sb("lb", [128, D]); ga = sb("ga", [128, D])
        P.dma("sync", lg[:], A["ln1_g"].to_broadcast([128, D]), writes=["lg"])
        P.dma("scalar", lb[:], A["ln1_b"].to_broadcast([128, D]), writes=["lb"])
        lnb = ln_bufs(K, ph)
        gy = sb("gy", [128, 4, L])
        att_t = [sb(f"att{i}", [128, D]) for i in range(2)]
        gm_t = [sb(f"gm{i}", [128, D]) for i in range(2)]
        gs_t = [sb(f"gs{i}", [128, D]) for i in range(2)]
        x_t = [sb(f"x{i}", [128, D]) for i in range(2)]
        sgb = [sb(f"sgb{i}", [128, D]) for i in range(2)]; s5o = [sb(f"s5o{i}", [128, D]) for i in range(2)]; mg = [sb(f"mg{i}", [128, D]) for i in range(2)]
        mT = sb("mT", [128, 8, 128])
        tmp = sb("tmp", [128, D]); rt = sb("rt", [128, D]); xn = sb("xn", [128, D])
        items = [(b, t) for b in range(K.nb) for t in range(K.ntile)]

        def stage_A(n):
            b, t = items[n]
            i = n % 2
            rows = slice(t * 128, (t + 1) * 128)
            if t == 0:
                P.dma("sync", gy[:], A["gyT"][b].rearrange("(j p) n -> p j n", p=128), reads=["d_gyT"], writes=["gy"])
            P.dma("sync", att_t[i][:], A["att"][b, rows, :], reads=["d_att"], writes=[f"att{i}"])
            P.dma("scalar", gm_t[i][:], A["gm"][b, rows, :], reads=["d_gm"], writes=[f"gm{i}"])
            P.dma("sync", gs_t[i][:], A["gs"][b, rows, :], reads=["d_gs"], writes=[f"gs{i}"])
            P.dma("scalar", x_t[i][:], A["x2"][b, rows, :], writes=[f"x{i}"])
            for ncn in range(4):
                pb = bank(K, ncn)
                for kc in range(4):
                    P.op("tensor", lambda e, pb=pb, kc=kc, ncn=ncn, rows=rows: e.matmul(pb[:, :], lhsT=gy[:, kc, rows], rhs=wglu[:, kc, ncn * 512:(ncn + 1) * 512], start=(kc == 0), stop=(kc == 3)),
                         reads=["gy", "wglu"], writes=[f"ps{ncn}"])
            P.op("scalar", lambda e: e.activation(out=sgb[i][:], in_=K.psum[:, 1024:2048], func=AF.Sigmoid), reads=["ps2", "ps3"], writes=[f"sgb{i}"])
            P.op("vector", lambda e: e.tensor_tensor(out=s5o[i][:], in0=K.psum[:, 0:1024], in1=sgb[i][:], op=ALU.mult), reads=["ps0", "ps1", f"sgb{i}"], writes=[f"s5o{i}"])
            P.op("scalar", lambda e: e.activation(out=gm_t[i][:], in_=gm_t[i][:], func=AF.Sigmoid), reads=[f"gm{i}"], writes=[f"gm{i}"])
            P.op("scalar", lambda e: e.activation(out=gs_t[i][:], in_=gs_t[i][:], func=AF.Sigmoid), reads=[f"gs{i}"], writes=[f"gs{i}"])
            P.op("gpsimd", lambda e: e.tensor_tensor(out=att_t[i][:], in0=att_t[i][:], in1=gm_t[i][:], op=ALU.mult), reads=[f"att{i}", f"gm{i}"], writes=[f"att{i}"])
            P.op("gpsimd", lambda e: e.tensor_tensor(out=s5o[i][:], in0=s5o[i][:], in1=gs_t[i][:], op=ALU.mult), reads=[f"s5o{i}", f"gs{i}"], writes=[f"s5o{i}"])
            P.op("vector", lambda e: e.tensor_tensor(out=mg[i][:], in0=att_t[i][:], in1=s5o[i][:], op=ALU.add), reads=[f"att{i}", f"s5o{i}"], writes=[f"mg{i}"])

        def stage_B(n):
            b, t = items[n]
            i = n % 2
            rows = slice(t * 128, (t + 1) * 128)
            if t == 0:
                P.dma("scalar", ga[:], A["modrows"][b:b + 1, 2048:3072].to_broadcast([128, D]), reads=["d_modrows"], writes=["ga"])
            for half in range(2):
                pb = bank(K, 4 + half)
                for jj in range(4):
                    kc = half * 4 + jj
                    P.op("tensor", lambda e, pb=pb, jj=jj, kc=kc: e.transpose(out=pb[:, jj * 128:(jj + 1) * 128], in_=mg[i][:, kc * 128:(kc + 1) * 128], identity=K.ident[:]),
                         reads=[f"mg{i}", "ident"], writes=[f"ps{4 + half}"])
                evac(K, mT[:, half * 4:(half + 1) * 4, :], pb[:, :], [f"ps{4 + half}"], ["mT"])
            for ncn in range(2):
                pb = bank(K, 6 + ncn)
                for kc in range(8):
                    P.op("tensor", lambda e, pb=pb, kc=kc, ncn=ncn: e.matmul(pb[:, :], lhsT=mT[:, kc, :], rhs=wout[:, kc, ncn * 512:(ncn + 1) * 512], start=(kc == 0), stop=(kc == 7)),
                         reads=["mT", "wout"], writes=[f"ps{6 + ncn}"])
            P.op("vector", lambda e: e.tensor_tensor(out=tmp[:], in0=K.psum[:, 3072:4096], in1=ga[:], op=ALU.mult), reads=["ps6", "ps7", "ga"], writes=["tmp"])
            P.op("vector", lambda e: e.scalar_tensor_tensor(out=rt[:], in0=x_t[i][:], scalar=float(ALPHA), in1=tmp[:], op0=ALU.mult, op1=ALU.add), reads=[f"x{i}", "tmp"], writes=["rt"])
            mean, rstd = layernorm_stats(K, lnb, rt, "rt", "p4")
            P.op("vector", lambda e: e.tensor_scalar(out=xn[:], in0=rt[:], scalar1=mean, scalar2=rstd, op0=ALU.subtract, op1=ALU.mult), reads=["rt", "p4mv", "p4rs2"], writes=["xn"])
            P.op("gpsimd", lambda e: e.tensor_tensor(out=xn[:], in0=xn[:], in1=lg[:], op=ALU.mult), reads=["xn", "lg"], writes=["xn"])
            P.op("gpsimd", lambda e: e.tensor_tensor(out=xn[:], in0=xn[:], in1=lb[:], op=ALU.add), reads=["xn", "lb"], writes=["xn"])
            P.dma("gpsimd", A["h1"][b, rows, :], xn[:], reads=["xn"], writes=["d_h1"])

        stage_A(0)
        for n in range(len(items)):
            if n + 1 < len(items):
                stage_A(n + 1)
            stage_B(n)
        end_phase(K)


def top16(K, x, xk, wk, sv_ap, si_ap, tag):
    P = K.P
    n = x.shape[1]
    P.op("vector", lambda e: e.max(out=sv_ap[:, 0:8], in_=x), reads=[xk], writes=[tag + "v0"])
    P.op("vector", lambda e: e.max_index(out=si_ap[:, 0:8], in_max=sv_ap[:, 0:8], in_values=x), reads=[xk, tag + "v0"], writes=[tag + "i0"])
    P.op("vector", lambda e: e.match_replace(out=wk[:, 0:n], in_to_replace=sv_ap[:, 0:8], in_values=x, imm_value=-1e30), reads=[xk, tag + "v0"], writes=["cwk"])
    P.op("vector", lambda e: e.max(out=sv_ap[:, 8:16], in_=wk[:, 0:n]), reads=["cwk"], writes=[tag + "v1"])
    P.op("vector", lambda e: e.max_index(out=si_ap[:, 8:16], in_max=sv_ap[:, 8:16], in_values=wk[:, 0:n]), reads=["cwk", tag + "v1"], writes=[tag + "i1"])
    return [tag + "v0", tag + "v1"], [tag + "i0", tag + "i1"]


def phase5(K):
    nc, P, A = K.nc, K.P, K.A
    NBUF = 8
    with ExitStack() as ph:
        sb = lambda n, s, dt=F32: ph.enter_context(nc.sbuf_tensor(f"q5_{n}", s, dt))
        wqq = [sb(f"wqq{i}", [128, 8, 256]) for i in range(2)]
        keysT = sb("keysT", [128, 16, 128]); P.dma("sync", keysT[:], A["keysT"], writes=["keysT"])
        io16 = sb("io16", [128, 16]); P.dma("sync", io16[:], A["iota16"][:, 0:16], writes=["io16"])
        lg = sb("lg", [128, D]); lb = sb("lb", [128, D])
        P.dma("sync", lg[:], A["ln2_g"].to_broadcast([128, D]), writes=["lg"])
        P.dma("scalar", lb[:], A["ln2_b"].to_broadcast([128, D]), writes=["lb"])
        shf = [sb(f"shf{b}", [128, D]) for b in range(K.nb)]
        scf = [sb(f"scf{b}", [128, D]) for b in range(K.nb)]
        gf = [sb(f"gf{b}", [128, D]) for b in range(K.nb)]
        for b in range(K.nb):
            P.dma("sync", shf[b][:], A["modrows"][b:b + 1, 3072:4096].to_broadcast([128, D]), reads=["d_modrows"], writes=[f"shf{b}"])
            P.dma("scalar", scf[b][:], A["modrows"][b:b + 1, 4096:5120].to_broadcast([128, D]), reads=["d_modrows"], writes=[f"scf{b}"])
            P.dma("sync", gf[b][:], A["modrows"][b:b + 1, 5120:6144].to_broadcast([128, D]), reads=["d_modrows"], writes=[f"gf{b}"])
            P.op("vector", lambda e, b=b: e.tensor_scalar(out=scf[b][:], in0=scf[b][:], scalar1=1.0, scalar2=None, op0=ALU.add), reads=[f"scf{b}"], writes=[f"scf{b}"])
        lnb = ln_bufs(K, ph)
        lnb2 = ln_bufs(K, ph)
        h1t = [sb(f"h1t{i}", [128, D]) for i in range(2)]
        xm = [sb(f"xm{i}", [128, D]) for i in range(2)]
        xmT = sb("xmT", [128, 8, 128])
        bufA = sb("bufA", [128, 2048]); bufB = sb("bufB", [128, 2048]); cwk = sb("cwk", [128, 256])
        sv = sb("sv", [128, 16, 16]); si = sb("si", [128, 16, 16], U32); sif = sb("sif", [128, 16, 16])
        tv_ = sb("tv", [128, 8, 16]); tp = sb("tp", [128, 8, 16], U32); tq = sb("tq", [128, 8, 16], U32)
        tif = sb("tif", [128, 128]); tjf = sb("tjf", [128, 128]); e0 = sb("e0", [128, 128]); e1 = sb("e1", [128, 128])
        eidx = [sb(f"eidx{i}", [128, 128], I32) for i in range(2)]
        ex = sb("ex", [128, 8, 16]); ssum = sb("ssum", [128, 16])
        gg = [sb(f"gg{i}", [128, 8, 16]) for i in range(2)]
        act = [sb(f"act{i}", [128, 128]) for i in range(2)]
        wgt = [sb(f"wgt{i}", [128, 128]) for i in range(2)]
        UV = [sb(f"UV{i}", [128, 2 * D]) for i in range(NBUF)]
        prod = [sb(f"prod{i}", [128, D]) for i in range(4)]
        accs = [sb(f"acc{i}", [128, D]) for i in range(4)]; r2 = sb("r2", [128, D])
        wqi = [0]

        def topk_gen(b, t, s):
            rows = slice(t * 128, (t + 1) * 128)
            hk, xk = f"h1t{s}", f"xm{s}"
            P.dma("sync", h1t[s][:], A["h1"][b, rows, :], reads=["d_h1"], writes=[hk])
            mean, rstd = layernorm_stats(K, lnb, h1t[s], hk, "p5")
            P.op("vector", lambda e, mean=mean, rstd=rstd: e.tensor_scalar(out=xm[s][:], in0=h1t[s][:], scalar1=mean, scalar2=rstd, op0=ALU.subtract, op1=ALU.mult), reads=[hk, "p5mv", "p5rs2"], writes=[xk])
            P.op("gpsimd", lambda e: e.tensor_tensor(out=xm[s][:], in0=xm[s][:], in1=scf[b][:], op=ALU.mult), reads=[xk, f"scf{b}"], writes=[xk])
            P.op("gpsimd", lambda e: e.tensor_tensor(out=xm[s][:], in0=xm[s][:], in1=shf[b][:], op=ALU.add), reads=[xk, f"shf{b}"], writes=[xk])
            yield
            for half in range(2):
                pb = bank(K, half)
                for jj in range(4):
                    kc = half * 4 + jj
                    P.op("tensor", lambda e, pb=pb, jj=jj, kc=kc: e.transpose(out=pb[:, jj * 128:(jj + 1) * 128], in_=xm[s][:, kc * 128:(kc + 1) * 128], identity=K.ident[:]),
                         reads=[xk, "ident"], writes=[f"ps{half}"])
                evac(K, xmT[:, half * 4:(half + 1) * 4, :], pb[:, :], [f"ps{half}"], ["xmT"])
            yield
            qT = bufA[:].rearrange("p (a s) -> p a s", s=128)
            for g4 in range(4):
                bi = 2 + g4 % 2
                pb = bank(K, bi)
                for h2 in range(2):
                    w = wqq[wqi[0] % 2]
                    wk_ = f"wqq{wqi[0] % 2}"
                    wqi[0] += 1
                    cb = g4 * 512 + h2 * 256
                    P.dma("scalar", w[:], A["peer_wq"].rearrange("(kc p) n -> p kc n", p=128)[:, :, cb:cb + 256], writes=[wk_])
                    for j2 in range(2):
                        jj = h2 * 2 + j2
                        for kc in range(8):
                            P.op("tensor", lambda e, pb=pb, jj=jj, j2=j2, kc=kc, w=w: e.matmul(pb[:, jj * 128:(jj + 1) * 128], lhsT=w[:, kc, j2 * 128:(j2 + 1) * 128], rhs=xmT[:, kc, :], start=(kc == 0), stop=(kc == 7)),
                                 reads=[wk_, "xmT"], writes=[f"ps{bi}"])
                evac(K, bufA[:, g4 * 512:(g4 + 1) * 512], pb[:, :], [f"ps{bi}"], ["bufA"])
                yield
            for g4 in range(4):
                bi = 4 + g4 % 2
                pb = bank(K, bi)
                for jj in range(4):
                    hz = g4 * 4 + jj
                    P.op("tensor", lambda e, pb=pb, jj=jj, hz=hz: e.matmul(pb[:, jj * 128:(jj + 1) * 128], lhsT=qT[:, hz, :], rhs=keysT[:, hz, :], start=True, stop=True),
                         reads=["bufA", "keysT"], writes=[f"ps{bi}"])
                evac(K, bufB[:, g4 * 512:(g4 + 1) * 512], pb[:, :], [f"ps{bi}"], ["bufB"])
            yield
            svk, sik = [], []
            for hz in range(16):
                a_, b_ = top16(K, bufB[:, hz * 128:(hz + 1) * 128], "bufB", cwk, sv[:, hz, :], si[:, hz, :], f"s1_{hz}_")
                svk += a_
                sik += b_
                if hz % 2 == 1:
                    yield
            P.op("vector", lambda e: e.tensor_copy(out=sif[:], in_=si[:]), reads=sik, writes=["sif"])
            sv4 = sv[:].rearrange("p (h z) k -> p h z k", z=2)
            sif4 = sif[:].rearrange("p (h z) k -> p h z k", z=2)
            cand4 = bufB[:].rearrange("p (h i j) -> p h i j", h=8, i=16)
            P.op("vector", lambda e: e.tensor_tensor(out=cand4, in0=sv4[:, :, 0, :].unsqueeze(3).to_broadcast([128, 8, 16, 16]), in1=sv4[:, :, 1, :].unsqueeze(2).to_broadcast([128, 8, 16, 16]), op=ALU.add),
                 reads=svk + ["bufB"], writes=["bufB"])
            yield
            tvk, tpk = [], []
            for h in range(8):
                a_, b_ = top16(K, bufB[:, h * 256:(h + 1) * 256], "bufB", cwk, tv_[:, h, :], tp[:, h, :], f"s2_{h}_")
                tvk += a_
                tpk += b_
                if h % 2 == 1:
                    yield
            tp2 = tp[:].rearrange("p h k -> p (h k)")
            tq2 = tq[:].rearrange("p h k -> p (h k)")
            P.op("vector", lambda e: e.tensor_single_scalar(out=tq2, in_=tp2, scalar=4, op=ALU.logical_shift_right), reads=tpk, writes=["tq"])
            P.op("vector", lambda e: e.tensor_copy(out=tif[:], in_=tq2), reads=["tq"], writes=["tif"])
            P.op("vector", lambda e: e.tensor_single_scalar(out=tq2, in_=tp2, scalar=15, op=ALU.bitwise_and), reads=tpk + ["tif"], writes=["tq"])
            P.op("vector", lambda e: e.tensor_copy(out=tjf[:], in_=tq2), reads=["tq"], writes=["tjf"])
            yield
            eq3 = bufA[:].rearrange("p (a i) -> p a i", i=16)
            eq4 = bufA[:].rearrange("p (h k i) -> p h k i", h=8, k=16)
            for (src, sk, z, dst, dk) in ((tif, "tif", 0, e0, "e0"), (tjf, "tjf", 1, e1, "e1")):
                P.op("vector", lambda e, src=src: e.tensor_tensor(out=eq3, in0=src[:].unsqueeze(2).to_broadcast([128, 128, 16]), in1=io16[:].unsqueeze(1).to_broadcast([128, 128, 16]), op=ALU.is_equal),
                     reads=[sk, "io16", "bufA"], writes=["bufA"])
                P.op("vector", lambda e, z=z: e.tensor_tensor(out=eq4, in0=eq4, in1=sif4[:, :, z, :].unsqueeze(2).to_broadcast([128, 8, 16, 16]), op=ALU.mult), reads=["bufA", "sif"], writes=["bufA"])
                P.op("vector", lambda e, dst=dst: e.tensor_reduce(out=dst[:], in_=eq3, axis=AX.X, op=ALU.add), reads=["bufA"], writes=[dk])
                yield
            P.op("vector", lambda e: e.scalar_tensor_tensor(out=e0[:], in0=e0[:], scalar=128.0, in1=e1[:], op0=ALU.mult, op1=ALU.add), reads=["e0", "e1"], writes=["e0"])
            P.op("vector", lambda e: e.tensor_copy(out=eidx[s][:], in_=e0[:]), reads=["e0"], writes=[f"eidx{s}"])
            P.op("vector", lambda e: e.tensor_tensor(out=ex[:], in0=tv_[:], in1=tv_[:, :, 0:1].to_broadcast([128, 8, 16]), op=ALU.subtract), reads=tvk, writes=["ex"])
            P.op("scalar", lambda e: e.activation(out=ex[:], in_=ex[:], func=AF.Exp), reads=["ex"], writes=["ex"])
            P.op("vector", lambda e: e.tensor_reduce(out=ssum[:, 0:8], in_=ex[:], axis=AX.X, op=ALU.add), reads=["ex"], writes=["ssum0"])
            P.op("vector", lambda e: e.reciprocal(out=ssum[:, 8:16], in_=ssum[:, 0:8]), reads=["ssum0"], writes=["ssum1"])
            P.op("vector", lambda e: e.tensor_tensor(out=gg[s][:], in0=ex[:], in1=ssum[:, 8:16].unsqueeze(2).to_broadcast([128, 8, 16]), op=ALU.mult), reads=["ex", "ssum1"], writes=[f"gg{s}"])
            yield

        def drain(g):
            if g is not None:
                for _ in g:
                    pass

        tiles = [(b, t) for b in range(K.nb) for t in range(K.ntile)]
        NTI = len(tiles)
        drain(topk_gen(tiles[0][0], tiles[0][1], 0))
        ring = [0]
        slots = {}
        issued = [0]
        state = {"nxt": None}

        def gather(s, c):
            i = ring[0] % NBUF
            ring[0] += 1
            P.dma_fn("gpsimd", lambda e, i=i, c=c, s=s: e.indirect_dma_start(out=UV[i][:, :], out_offset=None, in_=A["peer_uv"], in_offset=bass.IndirectOffsetOnAxis(ap=eidx[s][:, c:c + 1], axis=0)),
                     reads=[f"eidx{s}"], writes=[f"UV{i}"])
            return i

        def issue_upto(k):
            k = min(k, NTI * 128)
            while issued[0] < k:
                g = issued[0]
                ti_, c_ = divmod(g, 128)
                if c_ == 0 and ti_ > 0:
                    drain(state["nxt"])
                    state["nxt"] = None
                slots[g] = gather(ti_ % 2, c_)
                issued[0] += 1

        def prods(gp):
            ti_, pr = divmod(gp, 64)
            s = ti_ % 2
            c0 = pr * 2
            for kk in range(2):
                c = c0 + kk
                i = slots[ti_ * 128 + c]
                q = (gp % 2) * 2 + kk
                pk = f"prod{q}"
                P.op("vector", lambda e, i=i, q=q, s=s, c=c: e.scalar_tensor_tensor(out=prod[q][:], in0=UV[i][:, 0:D], scalar=1.0, in1=xm[s][:], op0=ALU.mult, op1=ALU.mult, accum_out=act[s][:, c:c + 1]),
                     reads=[f"UV{i}", f"xm{s}"], writes=[pk, f"actp{q}"])
            q0 = (gp % 2) * 2
            P.op("scalar", lambda e, c0=c0, s=s: e.activation(out=wgt[s][:, c0:c0 + 2], in_=act[s][:, c0:c0 + 2], func=AF.Gelu_apprx_tanh), reads=[f"actp{q0}", f"actp{q0 + 1}"], writes=[f"wgtp{gp % 2}"])

        def finish(gp):
            ti_, pr = divmod(gp, 64)
            s = ti_ % 2
            c0 = pr * 2
            gg2 = gg[s][:].rearrange("p h k -> p (h k)")
            P.op("vector", lambda e, c0=c0, s=s, gg2=gg2: e.tensor_tensor(out=wgt[s][:, c0:c0 + 2], in0=wgt[s][:, c0:c0 + 2], in1=gg2[:, c0:c0 + 2], op=ALU.mult), reads=[f"wgtp{gp % 2}", f"gg{s}"], writes=[f"wgtq{gp % 2}"])
            for kk in range(2):
                c = c0 + kk
                i = slots[ti_ * 128 + c]
                ak = f"acc{s}_{kk}"
                a_t = accs[s * 2 + kk]
                P.op("vector", lambda e, i=i, c=c, s=s, a_t=a_t: e.scalar_tensor_tensor(out=a_t[:], in0=UV[i][:, D:2 * D], scalar=wgt[s][:, c:c + 1], in1=a_t[:], op0=ALU.mult, op1=ALU.add),
                     reads=[f"UV{i}", f"wgtq{gp % 2}", ak], writes=[ak])
                issue_upto(ti_ * 128 + c + 1 + NBUF)

        def prologue(ti_):
            s = ti_ % 2
            for kk in range(2):
                P.op("gpsimd", lambda e, s=s, kk=kk: e.memset(accs[s * 2 + kk][:], 0.0), writes=[f"acc{s}_{kk}"])
            if ti_ + 1 < NTI:
                state["nxt"] = topk_gen(tiles[ti_ + 1][0], tiles[ti_ + 1][1], 1 - s)

        def epilogue(ti_):
            b, t = tiles[ti_]
            s = ti_ % 2
            rows = slice(t * 128, (t + 1) * 128)
            a0, a1 = accs[s * 2], accs[s * 2 + 1]
            P.op("vector", lambda e: e.tensor_tensor(out=a0[:], in0=a0[:], in1=a1[:], op=ALU.add), reads=[f"acc{s}_0", f"acc{s}_1"], writes=[f"acc{s}_0"])
            P.op("gpsimd", lambda e: e.tensor_tensor(out=r2[:], in0=a0[:], in1=gf[b][:], op=ALU.mult), reads=[f"acc{s}_0", f"gf{b}"], writes=["r2"])
            P.op("vector", lambda e: e.scalar_tensor_tensor(out=r2[:], in0=h1t[s][:], scalar=float(ALPHA), in1=r2[:], op0=ALU.mult, op1=ALU.add), reads=[f"h1t{s}", "r2"], writes=["r2"])
            mean, rstd = layernorm_stats(K, lnb2, r2, "r2", "p5b")
            P.op("vector", lambda e: e.tensor_scalar(out=r2[:], in0=r2[:], scalar1=mean, scalar2=rstd, op0=ALU.subtract, op1=ALU.mult), reads=["r2", "p5bmv", "p5brs2"], writes=["r2"])
            P.op("gpsimd", lambda e: e.tensor_tensor(out=r2[:], in0=r2[:], in1=lg[:], op=ALU.mult), reads=["r2", "lg"], writes=["r2"])
            P.op("gpsimd", lambda e: e.tensor_tensor(out=r2[:], in0=r2[:], in1=lb[:], op=ALU.add), reads=["r2", "lb"], writes=["r2"])
            P.dma("sync", A["out"][b, rows, :], r2[:], reads=["r2"], writes=["d_out"])

        prologue(0)
        issue_upto(NBUF)
        prods(0)
        NGP = NTI * 64
        for gp in range(NGP):
            ti_, pr = divmod(gp, 64)
            if pr == 0 and ti_ > 0:
                prologue(ti_)
            if gp + 1 < NGP:
                prods(gp + 1)
            finish(gp)
            if state["nxt"] is not None and pr % 2 == 1:
                try:
                    next(state["nxt"])
                except StopIteration:
                    state["nxt"] = None
            if pr == 63:
                epilogue(ti_)
        end_phase(K)


_CACHE = {}


def kernel(**inputs):
    inputs = {k: np.asarray(v) for k, v in inputs.items()}
    if "nc" not in _CACHE:
        _CACHE["nc"] = build()[0]
    nc = _CACHE["nc"]
    in_maps = [layout_inputs(inputs, c) for c in range(8)]
    res = run_bass_kernel_spmd(nc, in_maps, core_ids=list(range(8)))
    out = np.concatenate([np.asarray(r["out"]) for r in res.results], axis=0)
    return out.astype(np.float32)
```
